# Optimizing a Trainium2 kernel written in Bass

```python
import jax, jax.numpy as jnp
from jax import lax
import numpy as np

D_MODEL = 1024
BATCH = 2
SEQ = 8192
DEPTH = 2

MEM_LEN = 256
NSA_HEADS = 8
NSA_KV_HEADS = 2
NSA_HEAD_DIM = 64
NSA_REP = NSA_HEADS // NSA_KV_HEADS
CMP_BLOCK = 32
CMP_STRIDE = 16
SEL_BLOCK = 64
N_SELECT = 16
WINDOW = 512
Q_BLOCK = 128
NSA_WIDTH = NSA_HEADS * NSA_HEAD_DIM
KV_WIDTH = NSA_KV_HEADS * NSA_HEAD_DIM
GMLP_WIDTH = 256
GMLP_GROUPS = 4
GMLP_CHUNK = 128
CONV_WIDTH = 256
CONV_TAPS = 31
MIX_WIDTH = NSA_WIDTH + GMLP_WIDTH + CONV_WIDTH
IN_SPLITS = (NSA_WIDTH, KV_WIDTH, KV_WIDTH, KV_WIDTH, KV_WIDTH, KV_WIDTH, KV_WIDTH,
             NSA_HEADS * 3, 2 * GMLP_WIDTH, 2 * CONV_WIDTH)
IN_WIDTH = sum(IN_SPLITS)
XATTN_HEADS = 4
XATTN_HEAD_DIM = D_MODEL // XATTN_HEADS
D_FF = 2816
FFN_CONV_TAPS = 3

EPS = 1e-6
NEG_INF = -1e30
TINY = 1e-30
FORCE = 1e4

kernel_name = "hymba_style_nsa_gmlp_conformer_hybrid"


def _split_cols(z, widths):
    outs, off = [], 0
    for w in widths:
        outs.append(z[..., off:off + w])
        off += w
    return outs


def rms_norm(x, g):
    xf = x.astype(jnp.float32)
    y = xf * lax.rsqrt(jnp.mean(xf * xf, axis=-1, keepdims=True) + EPS)
    return (y * g.astype(jnp.float32)).astype(x.dtype)


def layer_norm(x, g, b):
    xf = x.astype(jnp.float32)
    mu = jnp.mean(xf, axis=-1, keepdims=True)
    var = jnp.mean(jnp.square(xf - mu), axis=-1, keepdims=True)
    y = (xf - mu) * lax.rsqrt(var + EPS)
    return (y * g.astype(jnp.float32) + b.astype(jnp.float32)).astype(x.dtype)


def masked_softmax(s, mask):
    s = jnp.where(mask, s, NEG_INF)
    m = jnp.max(s, axis=-1, keepdims=True)
    e = jnp.where(mask, jnp.exp(s - m), 0.0)
    return e / jnp.maximum(jnp.sum(e, axis=-1, keepdims=True), TINY)


def causal_depthwise_conv(x, w, b):
    taps = w.shape[0]
    y = lax.conv_general_dilated(x, w[:, None, :], window_strides=(1,), padding=[(taps - 1, 0)],
                                 dimension_numbers=("NWC", "WIO", "NWC"),
                                 feature_group_count=x.shape[-1])
    return y + b


def _to_heads(z, n):
    b, s, _ = z.shape
    return z.reshape(b, s, n, NSA_HEAD_DIM).transpose(0, 2, 1, 3)


def compress_blocks(kv, pe, w1, w2):
    b, g, s, dh = kv.shape
    n_cmp = (s - CMP_BLOCK) // CMP_STRIDE + 1
    idx = jnp.arange(n_cmp)[:, None] * CMP_STRIDE + jnp.arange(CMP_BLOCK)[None, :]
    blocks = kv[:, :, idx, :] + pe
    flat = blocks.reshape(b, g, n_cmp, CMP_BLOCK * dh)
    return jax.nn.gelu(flat @ w1) @ w2


def nsa_mixer(q, k_c_raw, v_c_raw, k_s, v_s, k_w, v_w, gate_logits, cmp_pe, cmp_w1, cmp_w2):
    b, s, _ = q.shape
    dh, G, R = NSA_HEAD_DIM, NSA_KV_HEADS, NSA_REP
    scale = dh ** -0.5
    k_cmp = compress_blocks(_to_heads(k_c_raw, G), cmp_pe[0], cmp_w1[0], cmp_w2[0])
    v_cmp = compress_blocks(_to_heads(v_c_raw, G), cmp_pe[1], cmp_w1[1], cmp_w2[1])
    n_cmp = k_cmp.shape[2]
    cmp_end = jnp.arange(n_cmp) * CMP_STRIDE + CMP_BLOCK - 1
    n_slc = s // SEL_BLOCK
    k_sel = min(N_SELECT, n_slc)
    k_slc_b = _to_heads(k_s, G).reshape(b, G, n_slc, SEL_BLOCK, dh)
    v_slc_b = _to_heads(v_s, G).reshape(b, G, n_slc, SEL_BLOCK, dh)
    ci = jnp.arange(n_cmp)[:, None] * CMP_STRIDE
    sj = jnp.arange(n_slc)[None, :] * SEL_BLOCK
    overlap = ((ci < sj + SEL_BLOCK) & (ci + CMP_BLOCK > sj)).astype(jnp.float32)
    pad = ((0, 0), (0, 0), (WINDOW, 0), (0, 0))
    k_win_p = jnp.pad(_to_heads(k_w, G), pad)
    v_win_p = jnp.pad(_to_heads(v_w, G), pad)
    nb = s // Q_BLOCK
    q_blocks = q.reshape(b, nb, Q_BLOCK, G, R, dh).transpose(1, 0, 3, 4, 2, 5)
    gather = jax.vmap(jax.vmap(lambda kb, ix: kb[ix]))

    def block_fn(args):
        qb, bi = args
        t = bi * Q_BLOCK + jnp.arange(Q_BLOCK)
        s_c = jnp.einsum("bgrtd,bgnd->bgrtn", qb, k_cmp).astype(jnp.float32) * scale
        p_c = masked_softmax(s_c, cmp_end[None, :] <= t[:, None])
        o_c = jnp.einsum("bgrtn,bgnd->bgrtd", p_c.astype(v_cmp.dtype), v_cmp)
        imp = jnp.einsum("bgtn,nj->bgtj", jnp.sum(p_c, axis=2), overlap)
        cur = (t // SEL_BLOCK)[:, None]
        jj = jnp.arange(n_slc)[None, :]
        forced = (jj == 0) | (jj == cur) | (jj == cur - 1)
        imp = jnp.where(forced, FORCE, imp)
        imp = jnp.where(jj <= cur, imp, -FORCE)
        _, sel = lax.top_k(imp, k_sel)
        gk = gather(k_slc_b, sel).reshape(b, G, Q_BLOCK, k_sel * SEL_BLOCK, dh)
        gv = gather(v_slc_b, sel).reshape(b, G, Q_BLOCK, k_sel * SEL_BLOCK, dh)
        kpos = (sel[..., None] * SEL_BLOCK + jnp.arange(SEL_BLOCK)).reshape(b, G, Q_BLOCK, k_sel * SEL_BLOCK)
        mask_s = (kpos <= t[None, None, :, None])[:, :, None]
        s_s = jnp.einsum("bgrtd,bgtmd->bgrtm", qb, gk).astype(jnp.float32) * scale
        p_s = masked_softmax(s_s, mask_s)
        o_s = jnp.einsum("bgrtm,bgtmd->bgrtd", p_s.astype(gv.dtype), gv)
        kw = lax.dynamic_slice_in_dim(k_win_p, bi * Q_BLOCK, Q_BLOCK + WINDOW, axis=2)
        vw = lax.dynamic_slice_in_dim(v_win_p, bi * Q_BLOCK, Q_BLOCK + WINDOW, axis=2)
        kp = (bi * Q_BLOCK - WINDOW + jnp.arange(Q_BLOCK + WINDOW))[None, :]
        mask_w = (kp <= t[:, None]) & (kp > t[:, None] - WINDOW) & (kp >= 0)
        s_w = jnp.einsum("bgrtd,bgkd->bgrtk", qb, kw).astype(jnp.float32) * scale
        p_w = masked_softmax(s_w, mask_w)
        o_w = jnp.einsum("bgrtk,bgkd->bgrtd", p_w.astype(vw.dtype), vw)
        return o_c, o_s, o_w

    o_c, o_s, o_w = lax.map(block_fn, (q_blocks, jnp.arange(nb)))
    def unblock(o):
        return o.transpose(1, 0, 4, 2, 3, 5).reshape(b, s, NSA_HEADS, dh)
    gates = jax.nn.sigmoid(gate_logits.reshape(b, s, NSA_HEADS, 3))
    o = (gates[..., 0:1] * unblock(o_c) + gates[..., 1:2] * unblock(o_s)
         + gates[..., 2:3] * unblock(o_w))
    return o.reshape(b, s, NSA_WIDTH)


def gmlp_mixer(z, ln_g, ln_b, w_s, b_s):
    b, s, _ = z.shape
    z = jax.nn.gelu(z)
    u, v = z[..., :GMLP_WIDTH], z[..., GMLP_WIDTH:]
    v = layer_norm(v, ln_g, ln_b)
    nc = s // GMLP_CHUNK
    v = v.reshape(b, nc, GMLP_CHUNK, GMLP_GROUPS, GMLP_WIDTH // GMLP_GROUPS)
    causal = jnp.tril(jnp.ones((GMLP_CHUNK, GMLP_CHUNK), dtype=w_s.dtype))
    mixed = jnp.einsum("gts,bcsgd->bctgd", w_s * causal, v) + b_s.T[:, :, None]
    return u * mixed.reshape(b, s, GMLP_WIDTH)


def conformer_conv_mixer(z, dw_w, dw_b, ln_g, ln_b):
    a, gt = z[..., :CONV_WIDTH], z[..., CONV_WIDTH:]
    h = a * jax.nn.sigmoid(gt)
    h = causal_depthwise_conv(h, dw_w, dw_b)
    h = layer_norm(h, ln_g, ln_b)
    return jax.nn.silu(h)


def memory_cross_attention(x, mem, g_x, g_m, wq, wk, wv, wo):
    b, s, _ = x.shape
    h = rms_norm(x, g_x)
    m = rms_norm(mem, g_m)
    q = (h @ wq).reshape(b, s, XATTN_HEADS, XATTN_HEAD_DIM)
    k = (m @ wk).reshape(b, m.shape[1], XATTN_HEADS, XATTN_HEAD_DIM)
    v = (m @ wv).reshape(b, m.shape[1], XATTN_HEADS, XATTN_HEAD_DIM)
    sc = jnp.einsum("bshd,bmhd->bhsm", q, k).astype(jnp.float32) * (XATTN_HEAD_DIM ** -0.5)
    p = jax.nn.softmax(sc, axis=-1)
    o = jnp.einsum("bhsm,bmhd->bshd", p.astype(v.dtype), v).reshape(b, s, D_MODEL)
    return o @ wo


def conv_ffn(x, g, w_up, dw_w, dw_b, w_down):
    h = causal_depthwise_conv(rms_norm(x, g) @ w_up, dw_w, dw_b)
    gate, up = h[..., :D_FF], h[..., D_FF:]
    return (jax.nn.silu(gate) * up) @ w_down


def setup_inputs(seed: int = 0) -> dict:
    key = jax.random.key(seed)
    ks = iter(jax.random.split(key, 40))
    def nrm(shape, scale):
        return jax.random.normal(next(ks), shape, jnp.float32) * scale
    def gain(shape):
        return 1.0 + 0.02 * jax.random.normal(next(ks), shape, jnp.float32)
    L, dh = DEPTH, NSA_HEAD_DIM
    return {
        "x": nrm((BATCH, SEQ, D_MODEL), 1.0),
        "mem": nrm((BATCH, MEM_LEN, D_MODEL), 1.0),
        "mix_norm_g": gain((L, D_MODEL)),
        "w_in": nrm((L, D_MODEL, IN_WIDTH), D_MODEL ** -0.5),
        "cmp_pe": nrm((L, 2, CMP_BLOCK, dh), 0.1),
        "cmp_w1": nrm((L, 2, CMP_BLOCK * dh, dh), (CMP_BLOCK * dh) ** -0.5),
        "cmp_w2": nrm((L, 2, dh, dh), dh ** -0.5),
        "gmlp_ln_g": gain((L, GMLP_WIDTH)),
        "gmlp_ln_b": nrm((L, GMLP_WIDTH), 0.02),
        "gmlp_ws": nrm((L, GMLP_GROUPS, GMLP_CHUNK, GMLP_CHUNK), GMLP_CHUNK ** -0.5),
        "gmlp_bs": gain((L, GMLP_GROUPS, GMLP_CHUNK)),
        "conv_dw_w": nrm((L, CONV_TAPS, CONV_WIDTH), CONV_TAPS ** -0.5),
        "conv_dw_b": nrm((L, CONV_WIDTH), 0.02),
        "conv_ln_g": gain((L, CONV_WIDTH)),
        "conv_ln_b": nrm((L, CONV_WIDTH), 0.02),
        "mix_out_g": gain((L, MIX_WIDTH)),
        "w_out": nrm((L, MIX_WIDTH, D_MODEL), MIX_WIDTH ** -0.5),
        "xattn_norm_g": gain((L, D_MODEL)),
        "mem_norm_g": gain((L, D_MODEL)),
        "xattn_wq": nrm((L, D_MODEL, D_MODEL), D_MODEL ** -0.5),
        "xattn_wk": nrm((L, D_MODEL, D_MODEL), D_MODEL ** -0.5),
        "xattn_wv": nrm((L, D_MODEL, D_MODEL), D_MODEL ** -0.5),
        "xattn_wo": nrm((L, D_MODEL, D_MODEL), D_MODEL ** -0.5),
        "ffn_norm_g": gain((L, D_MODEL)),
        "ffn_w_up": nrm((L, D_MODEL, 2 * D_FF), D_MODEL ** -0.5),
        "ffn_dw_w": nrm((L, FFN_CONV_TAPS, 2 * D_FF), FFN_CONV_TAPS ** -0.5),
        "ffn_dw_b": nrm((L, 2 * D_FF), 0.02),
        "ffn_w_down": nrm((L, D_FF, D_MODEL), D_FF ** -0.5),
        "final_norm_g": gain((D_MODEL,)),
    }


def reference(x, mem, mix_norm_g, w_in, cmp_pe, cmp_w1, cmp_w2, gmlp_ln_g, gmlp_ln_b, gmlp_ws,
              gmlp_bs, conv_dw_w, conv_dw_b, conv_ln_g, conv_ln_b, mix_out_g, w_out,
              xattn_norm_g, mem_norm_g, xattn_wq, xattn_wk, xattn_wv, xattn_wo,
              ffn_norm_g, ffn_w_up, ffn_dw_w, ffn_dw_b, ffn_w_down, final_norm_g):
    for l in range(DEPTH):
        z = rms_norm(x, mix_norm_g[l]) @ w_in[l]
        (q, kc, vc, ks, vs, kw, vw, gl, zg, zc) = _split_cols(z, IN_SPLITS)
        y_nsa = nsa_mixer(q, kc, vc, ks, vs, kw, vw, gl, cmp_pe[l], cmp_w1[l], cmp_w2[l])
        y_gmlp = gmlp_mixer(zg, gmlp_ln_g[l], gmlp_ln_b[l], gmlp_ws[l], gmlp_bs[l])
        y_conv = conformer_conv_mixer(zc, conv_dw_w[l], conv_dw_b[l], conv_ln_g[l], conv_ln_b[l])
        g_nsa, g_gmlp, g_conv = _split_cols(mix_out_g[l], (NSA_WIDTH, GMLP_WIDTH, CONV_WIDTH))
        y = jnp.concatenate([rms_norm(y_nsa, g_nsa), rms_norm(y_gmlp, g_gmlp),
                             rms_norm(y_conv, g_conv)], axis=-1)
        x = x + y @ w_out[l]
        x = x + memory_cross_attention(x, mem, xattn_norm_g[l], mem_norm_g[l], xattn_wq[l],
                                       xattn_wk[l], xattn_wv[l], xattn_wo[l])
        x = x + conv_ffn(x, ffn_norm_g[l], ffn_w_up[l], ffn_dw_w[l], ffn_dw_b[l], ffn_w_down[l])
    return rms_norm(x, final_norm_g)
```

```python
import numpy as np
from contextlib import ExitStack
import concourse.bass as bass
import concourse.mybir as mybir
from concourse.bass_utils import run_bass_kernel_spmd

F32 = mybir.dt.float32
BF16 = mybir.dt.bfloat16
AF = mybir.ActivationFunctionType
ALU = mybir.AluOpType
AX = mybir.AxisListType

NCORES = 8
D = 1024
S = 8192
NT = 16
TOK = NT * 128
IN_W = 2328
DFF = 2816
EPS = 1e-6
BIG = 30000.0
STOP_A = None
STOP_N = None


class Tr:
    __slots__ = ("w", "r")

    def __init__(self):
        self.w = {}
        self.r = {}


class V:
    __slots__ = ("ap", "main", "parts", "whole")

    def __init__(self, ap, main, parts, whole):
        self.ap, self.main, self.parts, self.whole = ap, main, parts, whole

    @property
    def trs(self):
        return [self.main] + list(self.parts)


class Buf:
    def __init__(self, t):
        self.t = t
        self.main = Tr()
        self.parts = {}

    def __getitem__(self, key):
        return V(self.t[key], self.main, list(self.parts.values()), True)

    def p(self, pk, key):
        if not isinstance(pk, (list, tuple)):
            pk = [pk]
        trs = []
        for q in pk:
            if q not in self.parts:
                self.parts[q] = Tr()
            trs.append(self.parts[q])
        return V(self.t[key], self.main, trs, False)


class Eng:
    def __init__(self, name, eng, sem, sid):
        self.name, self.eng, self.sem, self.sid = name, eng, sem, sid
        self.count = 0
        self.seen = {}


class KB:
    def __init__(self, ndma_sems=6):
        self.nc = bass.Bass("TRN2", target_bir_lowering=False)
        self.es = ExitStack()
        nc = self.nc
        self.sems = {}
        self.engs = {}
        for name, e in (("pe", nc.tensor), ("dve", nc.vector), ("act", nc.scalar),
                        ("pool", nc.gpsimd), ("sp", nc.sync)):
            sem = self.es.enter_context(nc.semaphore("s_" + name))
            sid = len(self.sems)
            self.sems[sid] = sem
            self.engs[name] = Eng(name, e, sem, sid)
        self.dq = {}
        for q in ("sp", "pool", "act"):
            lst = []
            for j in range(ndma_sems):
                sem = self.es.enter_context(nc.semaphore("d_%s%d" % (q, j)))
                sid = len(self.sems)
                self.sems[sid] = sem
                lst.append([sid, 0])
            self.dq[q] = [lst, 0]
        self.out_events = []
        self.nalloc = 0

    def sb(self, shape, dt=F32, name=None):
        self.nalloc += 1
        t = self.es.enter_context(self.nc.sbuf_tensor(name or "sb%d" % self.nalloc, list(shape), dt))
        return Buf(t)

    def ps(self, shape, dt=F32, name=None):
        self.nalloc += 1
        t = self.es.enter_context(self.nc.psum_tensor(name or "ps%d" % self.nalloc, list(shape), dt))
        return Buf(t)

    def dram(self, name, shape, dt=F32, kind="ExternalInput"):
        return self.nc.dram_tensor(name, list(shape), dt, kind=kind).ap()

    def _wait(self, E, deps):
        for sid, val in deps.items():
            if E.name == "pe" and sid == E.sid:
                continue
            if E.seen.get(sid, 0) >= val:
                continue
            E.eng.wait_ge(self.sems[sid], val)
            E.seen[sid] = val

    @staticmethod
    def _merge(d, o):
        for s, v in o.items():
            if d.get(s, 0) < v:
                d[s] = v

    def _deps(self, reads, writes):
        deps = {}
        for v in reads:
            if isinstance(v, V):
                for tr in v.trs:
                    self._merge(deps, tr.w)
        for v in writes:
            if isinstance(v, V):
                for tr in v.trs:
                    self._merge(deps, tr.w)
                    self._merge(deps, tr.r)
        return deps

    def _commit(self, ev, reads, writes):
        sid, val = ev
        for v in reads:
            if isinstance(v, V):
                for tr in ([v.main] if v.whole else v.parts):
                    if tr.r.get(sid, 0) < val:
                        tr.r[sid] = val
        for v in writes:
            if isinstance(v, V):
                if v.whole:
                    v.main.w = {sid: val}
                    v.main.r = {}
                    for tr in v.parts:
                        tr.w = {}
                        tr.r = {}
                else:
                    for tr in v.parts:
                        tr.w = {sid: val}
                        tr.r = {}

    def op(self, en, fn, reads, writes):
        E = self.engs[en]
        self._wait(E, self._deps(reads, writes))
        ins = fn(E.eng)
        E.count += 1
        ins.then_inc(E.sem, 1)
        self._commit((E.sid, E.count), reads, writes)

    @staticmethod
    def a(v):
        return v.ap if isinstance(v, V) else v

    def dma(self, q, out, in_, is_output=False, **kw):
        E = self.engs[q]
        lst, n = self.dq[q]
        slot = lst[n % len(lst)]
        self.dq[q][1] = n + 1
        if slot[1] > 0:
            self._wait(E, {slot[0]: slot[1]})
        self._wait(E, self._deps([in_], [out]))
        slot[1] += 16
        E.eng.dma_start(out=self.a(out), in_=self.a(in_), **kw).then_inc(self.sems[slot[0]], 16)
        ev = (slot[0], slot[1])
        self._commit(ev, [in_], [out])
        if is_output:
            self.out_events.append(ev)

    def mm(self, out, lhsT, rhs, start=True, stop=True):
        self.op("pe", lambda e: e.matmul(self.a(out), self.a(lhsT), self.a(rhs), start=start, stop=stop),
                [lhsT, rhs], [out])

    def tr(self, out, in_, ident):
        self.op("pe", lambda e: e.transpose(self.a(out), self.a(in_), self.a(ident)), [in_, ident], [out])

    def act(self, out, in_, func, bias=None, scale=None, accum=None, en="act"):
        kw = {}
        rd = [in_]
        if bias is not None:
            kw["bias"] = self.a(bias)
            rd.append(bias)
        if scale is not None:
            kw["scale"] = self.a(scale)
            rd.append(scale)
        wr = [out]
        if accum is not None:
            kw["accum_out"] = self.a(accum)
            wr.append(accum)
        self.op(en, lambda e: e.activation(out=self.a(out), in_=self.a(in_), func=func, **kw), rd, wr)

    def tt(self, en, out, a, b, op):
        self.op(en, lambda e: e.tensor_tensor(out=self.a(out), in0=self.a(a), in1=self.a(b), op=op), [a, b], [out])

    def ts(self, en, out, a, s1, s2=None, op0=ALU.mult, op1=None, accum=None):
        kw = {}
        if op1 is not None:
            kw["op1"] = op1
        wr = [out]
        if accum is not None:
            kw["accum_out"] = self.a(accum)
            wr.append(accum)
        self.op(en, lambda e: e.tensor_scalar(out=self.a(out), in0=self.a(a), scalar1=self.a(s1),
                                              scalar2=self.a(s2), op0=op0, **kw), [a, s1, s2], wr)

    def stt(self, en, out, a, s, b, op0, op1):
        self.op(en, lambda e: e.scalar_tensor_tensor(out=self.a(out), in0=self.a(a), scalar=self.a(s),
                                                     in1=self.a(b), op0=op0, op1=op1), [a, s, b], [out])

    def copy(self, en, out, in_):
        if en == "act":
            self.act(out, in_, AF.Copy)
        else:
            self.op(en, lambda e: e.tensor_copy(out=self.a(out), in_=self.a(in_)), [in_], [out])

    def memset(self, en, out, val):
        self.op(en, lambda e: e.memset(self.a(out), val), [], [out])

    def finish(self):
        E = self.engs["sp"]
        deps = {}
        for sid, val in self.out_events:
            if deps.get(sid, 0) < val:
                deps[sid] = val
        self._wait(E, deps)
        self.es.close()
        return self.nc


def rms_transpose(k, xt, gcol, ident, dstT, tcol, work, pst):
    junk, ssq, rstd, xs = work["junk"], work["ssq"], work["rstd"], work["xs"]
    k.act(junk[:, :], xt, AF.Square, accum=ssq[:, :])
    k.ts("dve", rstd[:, :], ssq[:, :], 1.0 / D, EPS, op0=ALU.mult, op1=ALU.add)
    k.act(rstd[:, :], rstd[:, :], AF.Sqrt)
    k.op("dve", lambda e: e.reciprocal(out=rstd.t[:, :], in_=rstd.t[:, :]), [rstd[:, :]], [rstd[:, :]])
    k.ts("dve", xs[:, :], xt, rstd[:, :], None, op0=ALU.mult)
    for h in range(2):
        for j in range(4):
            c = 4 * h + j
            k.tr(pst[h][:, 128 * j:128 * j + 128], xs[:, 128 * c:128 * c + 128], ident[:, :])
    for h in range(2):
        for j in range(4):
            c = 4 * h + j
            dv = dstT(c, tcol)
            k.act(dv, pst[h][:, 128 * j:128 * j + 128], AF.Copy, scale=gcol[:, c:c + 1])


def build_A():
    k = KB()
    x = k.dram("x", [NT, 128, D])
    w = k.dram("w_in", [D, IN_W])
    gcol_d = k.dram("gcol", [128, 8])
    ident_d = k.dram("ident", [128, 128])
    qT = k.dram("qT", [512, TOK], kind="ExternalOutput")
    KTc = k.dram("KTc", [256, TOK], kind="ExternalOutput")
    KTs = k.dram("KTs", [256, TOK], BF16, kind="ExternalOutput")
    VV = k.dram("VV", [TOK, 256], BF16, kind="ExternalOutput")
    GZ = k.dram("GZ", [TOK, 536], kind="ExternalOutput")
    HT = k.dram("HT", [256, TOK], kind="ExternalOutput")

    ident = k.sb([128, 128]); gcol = k.sb([128, 8])
    k.dma("sp", ident[:, :], ident_d[:, :])
    k.dma("sp", gcol[:, :], gcol_d[:, :])
    xnT = k.sb([128, 8, TOK], BF16, name="xnT")
    xts = [k.sb([128, D]) for _ in range(2)]
    work = dict(junk=k.sb([128, D]), ssq=k.sb([128, 1]), rstd=k.sb([128, 1]), xs=k.sb([128, D]))
    pst = [k.ps([128, 512]) for _ in range(2)]
    for i in range(NT):
        xt = xts[i % 2]
        k.dma("sp", xt[:, :], x[i, :, :])
        rms_transpose(k, xt[:, :], gcol, ident, lambda c, tc, i=i: xnT.p(i, (slice(None), c, slice(tc, tc + 128))),
                      128 * i, work, pst)

    wv = w.rearrange("(c p) n -> p c n", p=128)
    wbufs = [k.sb([128, 8, 512], BF16) for _ in range(2)]
    wstgA = [k.sb([128, 8, 512]) for _ in range(2)]
    pacc = [k.ps([128, 512]) for _ in range(4)]
    stg = [k.sb([128, 512]) for _ in range(3)]
    stgb = [k.sb([128, 512], BF16) for _ in range(3)]
    sg = k.sb([128, 512])
    cnt = {"w": 0, "p": 0, "s": 0}

    def loadw(c0, c1):
        wb = wbufs[cnt["w"] % 2]; ws = wstgA[cnt["w"] % 2]; cnt["w"] += 1
        k.dma("sp", ws[:, :, 0:c1 - c0], wv[:, :, c0:c1])
        k.copy("pool", wb[:, :, 0:c1 - c0], ws[:, :, 0:c1 - c0])
        return wb

    def nextp():
        p = pacc[cnt["p"] % 4]; cnt["p"] += 1
        return p

    def nexts(bf=False):
        s = (stgb if bf else stg)[cnt["s"] % 3]; cnt["s"] += 1
        return s

    def feat_block(wb, j0, tb):
        p = nextp()
        for kc in range(8):
            k.mm(p[:, :], wb[:, kc, j0:j0 + 128],
                 xnT.p([4 * tb + u for u in range(4)], (slice(None), kc, slice(512 * tb, 512 * tb + 512))),
                 start=(kc == 0), stop=(kc == 7))
        return p

    def tok_block(wb, j0, n, i):
        p = nextp()
        for kc in range(8):
            k.mm(p[:, 0:n], xnT.p(i, (slice(None), kc, slice(128 * i, 128 * i + 128))), wb[:, kc, j0:j0 + n],
                 start=(kc == 0), stop=(kc == 7))
        return p

    wb = loadw(0, 512)
    for j in range(4):
        for tb in range(4):
            p = feat_block(wb, 128 * j, tb)
            s = nexts()
            k.act(s[:, :], p[:, :], AF.Copy, scale=0.125)
            k.dma("pool", qT[128 * j:128 * j + 128, 512 * tb:512 * tb + 512], s[:, :], is_output=True)
    if STOP_A == "B1":
        return k.finish()
    wb = loadw(512, 1024)
    for j in range(3):
        for tb in range(4):
            p = feat_block(wb, 128 * j, tb)
            s = nexts(bf=(j == 2))
            k.copy("dve", s[:, :], p[:, :])
            dst = KTc[128 * j:128 * j + 128, 512 * tb:512 * tb + 512] if j < 2 else KTs[0:128, 512 * tb:512 * tb + 512]
            k.dma("pool" if j < 2 else "sp", dst, s[:, :], is_output=True)
    for i in range(NT):
        p = tok_block(wb, 384, 128, i)
        s = nexts(bf=True)
        k.copy("dve", s[:, 0:128], p[:, 0:128])
        k.dma("sp", VV[128 * i:128 * i + 128, 0:128], s[:, 0:128], is_output=True)
    if STOP_A == "B2":
        return k.finish()
    wb = loadw(1024, 1304)
    for tb in range(4):
        p = feat_block(wb, 0, tb)
        s = nexts(bf=True)
        k.copy("dve", s[:, :], p[:, :])
        k.dma("sp", KTs[128:256, 512 * tb:512 * tb + 512], s[:, :], is_output=True)
    if STOP_A == "B2a":
        return k.finish()
    for i in range(NT):
        if STOP_A == "B2b" and i == 1:
            return k.finish()
        p = tok_block(wb, 128, 152, i)
        s = nexts(bf=True)
        k.copy("dve", s[:, 0:128], p[:, 0:128])
        k.dma("sp", VV[128 * i:128 * i + 128, 128:256], s[:, 0:128], is_output=True)
        s2 = nexts()
        k.copy("dve", s2[:, 0:24], p[:, 128:152])
        k.dma("pool", GZ[128 * i:128 * i + 128, 0:24], s2[:, 0:24], is_output=True)
    if STOP_A == "B3":
        return k.finish()
    wb = loadw(1304, 1816)
    for i in range(NT):
        p = tok_block(wb, 0, 512, i)
        s = nexts()
        k.copy("dve", s[:, :], p[:, :])
        k.dma("pool", GZ[128 * i:128 * i + 128, 24:536], s[:, :], is_output=True)
    if STOP_A == "B4":
        return k.finish()
    wb = loadw(1816, 2328)
    for j in range(2):
        for tb in range(4):
            pa = feat_block(wb, 128 * j, tb)
            pg = feat_block(wb, 256 + 128 * j, tb)
            k.act(sg[:, :], pg[:, :], AF.Sigmoid)
            s = nexts()
            k.tt("dve", s[:, :], pa[:, :], sg[:, :], ALU.mult)
            k.dma("pool", HT[128 * j:128 * j + 128, 512 * tb:512 * tb + 512], s[:, :], is_output=True)
    return k.finish()


_PROGS = {}


def _prog(name, builder):
    if name not in _PROGS:
        _PROGS[name] = builder()
    return _PROGS[name]


def _run(name, builder, in_maps):
    nc = _prog(name, builder)
    res = run_bass_kernel_spmd(nc, in_maps, core_ids=list(range(NCORES)))
    return res.results


def own_tiles(a, core):
    b, sq = core // 4, core % 4
    t = a[b].reshape((64, 128) + a.shape[2:])
    return np.ascontiguousarray(t[sq::4])


def gcol_of(g):
    return np.ascontiguousarray(g.reshape(8, 128).T)


IDENT = np.eye(128, dtype=np.float32)


def run_A(x, w_in_l, g_l):
    maps = []
    for c in range(NCORES):
        maps.append({"x": own_tiles(x, c), "w_in": np.ascontiguousarray(w_in_l), "gcol": gcol_of(g_l),
                     "ident": IDENT})
    return _run("A", build_A, maps)


ND = BF16
TINY = 1e-30


def v3(buf, view2d_t, r=4):
    return V(view2d_t.rearrange("p (r t) -> p r t", r=r), buf.main, list(buf.parts.values()), True)


def bc3(buf, t2d, n, axis):
    if axis == 1:
        ap = t2d.unsqueeze(1).to_broadcast([t2d.shape[0], n, t2d.shape[1]])
    else:
        ap = t2d.unsqueeze(2).to_broadcast([t2d.shape[0], t2d.shape[1], n])
    return V(ap, buf.main, list(buf.parts.values()), True)


def build_N():
    k = KB()
    qT_d = k.dram("qT", [512, TOK])
    KTc_d = k.dram("KTc", [256, S])
    KTs_d = k.dram("KTs", [256, S], ND)
    VV_d = k.dram("VV", [S, 256], ND)
    GL_d = k.dram("GL", [TOK, 24])
    pe_d = k.dram("peT", [64, 2, 32])
    w1_d = k.dram("w1", [2, 64, 32, 64])
    w2_d = k.dram("w2", [2, 64, 64])
    cmask_d = k.dram("cmask", [NT, 4, 128, 128])
    smask_d = k.dram("smask", [128, 4, 128], ND)
    wmask_d = k.dram("wmask", [128, 8, 128], ND)
    keep_d = k.dram("keepm", [NT, 128, 128])
    add_d = k.dram("addm", [NT, 128, 128])
    ovl_d = k.dram("ovl", [128, 4, 128])
    i4_d = k.dram("i4b", [128, 512], ND)
    identN_d = k.dram("ident", [128, 128])
    YN = k.dram("YN", [TOK, 512], kind="ExternalOutput")

    ksT = k.sb([128, S], ND, "ksT"); kwT = k.sb([128, S], ND, "kwT")
    vs1 = k.sb([128, 64, 2, 65], ND, "vs1"); vw1 = k.sb([128, 64, 2, 65], ND, "vw1")
    kcv = k.sb([128, S], F32, "kcv")
    w1sb = k.sb([128, 32, 64]); peT = k.sb([64, 2, 32]); w2k = k.sb([64, 2, 128]); w2v = k.sb([64, 64])
    kcmpT = k.sb([128, 512]); hidT = k.sb([64, 512]); bias_sb = k.sb([64, 1])
    rhs_c = k.sb([128, 2, 4, 193])
    smask = k.sb([128, 4, 128], ND); wmask = k.sb([128, 8, 128], ND); i4b = k.sb([128, 512], ND)
    negX = k.sb([128, 128, 64], ND, "negX")
    Eb = [k.sb([128, 512]) for _ in range(4)]
    Pb = [k.sb([128, 512], ND) for _ in range(3)]
    cmb = [k.sb([128, 128]) for _ in range(2)]
    kpb = [k.sb([128, 128]) for _ in range(2)]; adb = [k.sb([128, 128]) for _ in range(2)]
    q32b = [k.sb([128, 4, 128]) for _ in range(2)]; qbb = [k.sb([128, 512], ND) for _ in range(2)]
    glb = [k.sb([128, 24]) for _ in range(2)]; sigb = [k.sb([128, 24]) for _ in range(2)]
    ynb = [k.sb([128, 512]) for _ in range(2)]
    imp = k.sb([128, 128]); imp2 = k.sb([128, 128]); wk = k.sb([128, 128]); m8 = k.sb([128, 16])
    den4 = k.sb([128, 4]); coef = k.sb([128, 4])
    pbig = [k.ps([128, 512]) for _ in range(3)]
    pc = k.ps([128, 2, 512]); pso = k.ps([128, 512]); poT = [k.ps([128, 512]) for _ in range(2)]
    psw = poT[0]
    oT_sb = k.sb([65, 512]); identN = k.sb([128, 128])
    k.dma("sp", identN[:, :], identN_d[:, :])
    cnt = {"p": 0, "P": 0, "cm": 0}

    def nextp():
        p = pbig[cnt["p"] % 3]; cnt["p"] += 1
        return p

    k.dma("sp", peT[:, :, :], pe_d[:, :, :])
    k.memset("pool", w2k[:, :, :], 0.0)
    k.dma("sp", w2k[:, 0, 0:64], w2_d[0, :, :])
    k.dma("sp", w2k[:, 1, 64:128], w2_d[0, :, :])
    k.dma("sp", w2v[:, :], w2_d[1, :, :])
    k.dma("sp", smask[:, :, :], smask_d[:, :, :])
    k.dma("sp", wmask[:, :, :], wmask_d[:, :, :])
    k.dma("sp", i4b[:, :], i4_d[:, :])
    k.memset("pool", rhs_c[:, :, :, 192:193], 1.0)
    for g in range(2):
        k.dma("sp", rhs_c[:, g, :, 0:128], ovl_d[:, :, :])
    k.memset("dve", kcmpT[:, :], 0.0)
    k.memset("dve", hidT[:, :], 0.0)

    if STOP_N == "const":
        return k.finish()
    for kv in range(2):
        for q4 in range(4):
            k.dma("sp", kcv[:, 2048 * q4:2048 * q4 + 2048], KTc_d[128 * kv:128 * kv + 128, 2048 * q4:2048 * q4 + 2048])
        for half in range(2):
            k.dma("pool", w1sb[64 * half:64 * half + 64, :, :], w1_d[kv, :, :, :])
        bp = pso
        for l in range(32):
            k.mm(bp[0:64, 0:1], w1sb[0:64, l, :], peT[:, kv, l:l + 1], start=(l == 0), stop=(l == 31))
        k.copy("dve", bias_sb[:, :], bp[0:64, 0:1])
        for g in range(2):
            hp = nextp()
            for l in range(32):
                k.mm(hp[0:64, 0:511], w1sb[64 * g:64 * g + 64, l, :], kcv[64 * g:64 * g + 64, l:l + 8161:16],
                     start=(l == 0), stop=(l == 31))
            k.act(hidT[:, 0:511], hp[0:64, 0:511], AF.Gelu, bias=bias_sb[:, 0:1])
            if kv == 0:
                op_ = nextp()
                k.mm(op_[:, 0:511], w2k[:, g, :], hidT[:, 0:511])
                k.copy("dve", kcmpT[64 * g:64 * g + 64, 0:511], op_[64 * g:64 * g + 64, 0:511])
            else:
                for cc in range(4):
                    k.mm(psw[:, 64 * cc:64 * cc + 64], hidT[:, 128 * cc:128 * cc + 128], w2v[:, :])
                for cc in range(4):
                    k.copy("dve", rhs_c[:, g, cc, 128:192], psw[:, 64 * cc:64 * cc + 64])

    if STOP_N == "cmp":
        return k.finish()
    for q4 in range(4):
        sl = slice(2048 * q4, 2048 * q4 + 2048)
        k.dma("sp", ksT[:, sl], KTs_d[0:128, sl])
    VVr = VV_d.rearrange("(c p) f -> p c f", p=128)
    k.memset("pool", vs1[:, :, :, 64:65], 1.0)
    k.memset("pool", vw1[:, :, :, 64:65], 1.0)
    for q4 in range(4):
        cs = slice(16 * q4, 16 * q4 + 16)
        for g in range(2):
            k.dma("sp", vs1[:, cs, g, 0:64], VVr[:, cs, 64 * g:64 * g + 64])
    for q4 in range(4):
        sl = slice(2048 * q4, 2048 * q4 + 2048)
        k.dma("sp", kwT[:, sl], KTs_d[128:256, sl])
    for q4 in range(4):
        cs = slice(16 * q4, 16 * q4 + 16)
        for g in range(2):
            k.dma("sp", vw1[:, cs, g, 0:64], VVr[:, cs, 128 + 64 * g:128 + 64 * g + 64])

    if STOP_N == "load":
        return k.finish()
    def untranspose(po):
        k.copy("dve", oT_sb[:, :], po[0:65, :])
        for r in range(4):
            k.tr(pso[:, 65 * r:65 * r + 65], oT_sb[:, 128 * r:128 * r + 128], identN[0:65, 0:65])

    def branch_out(ps_t, stride, g, gate_idx, sig, yn, first):
        for r in range(4):
            k.ts("dve", den4[:, r:r + 1], ps_t(r, 64, 65), TINY, None, op0=ALU.max)
        k.op("dve", lambda e: e.reciprocal(out=den4.t[:, :], in_=den4.t[:, :]), [den4[:, :]], [den4[:, :]])
        k.tt("dve", coef[:, :], sig[:, 12 * g + gate_idx:12 * g + 12:3], den4[:, :], ALU.mult)
        for r in range(4):
            h = 4 * g + r
            if first:
                k.ts("dve", yn[:, 64 * h:64 * h + 64], ps_t(r, 0, 64), coef[:, r:r + 1], None, op0=ALU.mult)
            else:
                k.stt("dve", yn[:, 64 * h:64 * h + 64], ps_t(r, 0, 64), coef[:, r:r + 1], yn[:, 64 * h:64 * h + 64],
                      ALU.mult, ALU.add)

    negXs = [negX, k.sb([128, 128, 64], ND, "negX1")]
    imps = [imp, k.sb([128, 128])]; imp2s = [imp2, k.sb([128, 128])]; wks = [wk, k.sb([128, 128])]
    m8s = [m8, k.sb([128, 16])]; den4c = [k.sb([128, 4]) for _ in range(2)]; coefc = [k.sb([128, 4]) for _ in range(2)]

    def bufs(i):
        return q32b[i % 2], qbb[i % 2], glb[i % 2], sigb[i % 2], ynb[i % 2], kpb[i % 2], adb[i % 2]

    def slot_loads(i):
        q32, qb, gl, sig, yn, kp, ad = bufs(i)
        for g in range(2):
            k.dma("sp", q32[64 * g:64 * g + 64, :, :],
                  qT_d[256 * g:256 * g + 256, 128 * i:128 * i + 128].rearrange("(r d) t -> d r t", d=64))
        k.dma("sp", gl[:, :], GL_d[128 * i:128 * i + 128, :])
        k.dma("sp", kp[:, :], keep_d[i, :, :])
        k.dma("sp", ad[:, :], add_d[i, :, :])
        q2d = V(q32.t[:, :, :].rearrange("p r t -> p (r t)"), q32.main, [], True)
        k.copy("dve", qb[:, :], q2d)
        k.act(sig[:, :], gl[:, :], AF.Sigmoid)

    def cmp_phase(i, g):
        q32, qb, gl, sig, yn, kp, ad = bufs(i)
        imp_, imp2_, wk_, m8_, d4, cf, nX = imps[g], imp2s[g], wks[g], m8s[g], den4c[g], coefc[g], negXs[g]
        ncc = (32 * i + 30) // 128 + 1
        gs = slice(64 * g, 64 * g + 64)
        q2g = V(q32.t[gs, :, :].rearrange("p r t -> p (r t)"), q32.main, [], True)
        for cc in range(ncc):
            cm = cmb[cnt["cm"] % 2]; cnt["cm"] += 1
            k.dma("pool", cm[:, :], cmask_d[i, cc, :, :])
            sc = nextp()
            k.mm(sc[:, :], kcmpT[gs, 128 * cc:128 * cc + 128], q2g)
            E = Eb[cc]
            k.act(E[:, :], sc[:, :], AF.Exp)
            k.tt("dve", v3(E, E.t[:, :]), v3(E, E.t[:, :]), bc3(cm, cm.t[:, :], 4, 1), ALU.mult)
        for r in range(4):
            for cc in range(ncc):
                k.mm(pc[:, r // 2, 193 * (r % 2):193 * (r % 2) + 193], Eb[cc][:, 128 * r:128 * r + 128],
                     rhs_c[:, g, cc, :], start=(cc == 0), stop=(cc == ncc - 1))
        for r in range(4):
            k.ts("dve", d4[:, r:r + 1], pc[:, r // 2, 193 * (r % 2) + 192:193 * (r % 2) + 193], TINY, None, op0=ALU.max)
        recip(k, d4[:, :])
        k.ts("dve", imp_[:, :], pc[:, 0, 0:128], d4[:, 0:1], None, op0=ALU.mult)
        for r in range(1, 4):
            k.stt("dve", imp_[:, :], pc[:, r // 2, 193 * (r % 2):193 * (r % 2) + 128], d4[:, r:r + 1], imp_[:, :],
                  ALU.mult, ALU.add)
        k.tt("dve", cf[:, :], sig[:, 12 * g:12 * g + 12:3], d4[:, :], ALU.mult)
        for r in range(4):
            h = 4 * g + r
            k.ts("dve", yn[:, 64 * h:64 * h + 64], pc[:, r // 2, 193 * (r % 2) + 128:193 * (r % 2) + 192],
                 cf[:, r:r + 1], None, op0=ALU.mult)
        k.tt("dve", imp2_[:, :], imp_[:, :], kp[:, :], ALU.mult)
        k.tt("dve", imp2_[:, :], imp2_[:, :], ad[:, :], ALU.add)
        k.op("dve", lambda e: e.max(out=m8_.t[:, 0:8], in_=imp2_.t[:, :]), [imp2_[:, :]], [m8_[:, :]])
        k.op("dve", lambda e: e.match_replace(out=wk_.t[:, :], in_to_replace=m8_.t[:, 0:8], in_values=imp2_.t[:, :],
                                              imm_value=-1e30), [m8_[:, :], imp2_[:, :]], [wk_[:, :]])
        k.op("dve", lambda e: e.max(out=m8_.t[:, 8:16], in_=wk_.t[:, :]), [wk_[:, :]], [m8_[:, :]])
        k.ts("dve", nX[:, :, :], bc3(imp2_, imp2_.t[:, :], 64, 2), m8_[:, 15:16], 1.0, op0=ALU.is_ge, op1=ALU.subtract)

    def attn_phase(i, g):
        q32, qb, gl, sig, yn, kp, ad = bufs(i)
        nX = negXs[g]
        gs = slice(64 * g, 64 * g + 64)
        nch = 4 * i + 4
        prev = None
        for c in range(nch):
            sp_ = nextp()
            k.mm(sp_[:, :], ksT[gs, 128 * c:128 * c + 128], qb[gs, :], start=True, stop=False)
            k.mm(sp_[:, :], V(nX.t[:, 2 * c:2 * c + 2, :].rearrange("p a b -> p (a b)"), nX.main, [], True),
                 i4b[:, :], start=False, stop=True)
            P = Pb[cnt["P"] % 3]; cnt["P"] += 1
            k.act(P[:, :], sp_[:, :], AF.Exp)
            if c >= 4 * i:
                k.tt("dve", v3(P, P.t[:, :]), v3(P, P.t[:, :]), bc3(smask, smask.t[:, c - 4 * i, :], 4, 1), ALU.mult)
            if prev is not None:
                k.mm(poT[0][0:65, :], vs1[:, prev[0], g, :], prev[1][:, :], start=(prev[0] == 0), stop=False)
            prev = (c, P)
        k.mm(poT[0][0:65, :], vs1[:, prev[0], g, :], prev[1][:, :], start=(prev[0] == 0), stop=True)
        untranspose(poT[0])
        branch_out(lambda r, a, b: pso[:, 65 * r + a:65 * r + b], 65, g, 1, sig, yn, False)
        cl = [c for c in range(4 * i - 4, 4 * i + 4) if c >= 0]
        prev = None
        for c in cl:
            rp = c - (4 * i - 4)
            sp_ = nextp()
            k.mm(sp_[:, :], kwT[gs, 128 * c:128 * c + 128], qb[gs, :])
            P = Pb[cnt["P"] % 3]; cnt["P"] += 1
            k.act(P[:, :], sp_[:, :], AF.Exp)
            k.tt("dve", v3(P, P.t[:, :]), v3(P, P.t[:, :]), bc3(wmask, wmask.t[:, rp, :], 4, 1), ALU.mult)
            if prev is not None:
                k.mm(poT[1][0:65, :], vw1[:, prev[0], g, :], prev[1][:, :], start=(prev[0] == cl[0]), stop=False)
            prev = (c, P)
        k.mm(poT[1][0:65, :], vw1[:, prev[0], g, :], prev[1][:, :], start=(prev[0] == cl[0]), stop=True)
        untranspose(poT[1])
        branch_out(lambda r, a, b: pso[:, 65 * r + a:65 * r + b], 65, g, 2, sig, yn, False)

    slot_loads(0)
    cmp_phase(0, 0)
    cmp_phase(0, 1)
    for i in range(NT):
        attn_phase(i, 0)
        if i + 1 < NT:
            slot_loads(i + 1)
            cmp_phase(i + 1, 0)
        attn_phase(i, 1)
        if i + 1 < NT:
            cmp_phase(i + 1, 1)
        k.dma("pool", YN[128 * i:128 * i + 128, :], ynb[i % 2][:, :], is_output=True)
    return k.finish()


def nsa_masks(sq):
    import ml_dtypes
    n_l = np.arange(128)[:, None]; tt = np.arange(128)[None, :]
    cmask = np.zeros((NT, 4, 128, 128), np.float32)
    for i in range(NT):
        bi = 4 * i + sq
        for cc in range(4):
            n = 128 * cc + n_l
            cmask[i, cc] = ((n <= 510) & (16 * n + 31 <= 128 * bi + tt)).astype(np.float32)
    kk = n_l
    smask = np.zeros((128, 4, 128), np.float32)
    for r in range(4):
        if r < sq:
            smask[:, r, :] = 1.0
        elif r == sq:
            smask[:, r, :] = (kk <= tt)
    wmask = np.zeros((128, 8, 128), np.float32)
    for rp in range(8):
        d = rp - sq
        if d == 0:
            wmask[:, rp, :] = (kk > tt)
        elif 1 <= d <= 3:
            wmask[:, rp, :] = 1.0
        elif d == 4:
            wmask[:, rp, :] = (kk <= tt)
    keep = np.ones((NT, 128, 128), np.float32); add = np.zeros((NT, 128, 128), np.float32)
    jj = np.arange(128)[None, :]
    for i in range(NT):
        bi = 4 * i + sq
        cur = (2 * bi + (np.arange(128) >= 64))[:, None]
        val = np.zeros((128, 128), np.float32); frc = np.zeros((128, 128), bool)
        m0 = np.broadcast_to(jj == 0, (128, 128)); val[m0] = 1e4; frc |= m0
        m1 = (jj == cur - 1); val[m1] = 1e4 + 1; frc |= m1
        m2 = (jj == cur); val[m2] = 1e4 + 2; frc |= m2
        fut = (jj > cur); val[fut] = -1e4; frc |= fut
        keep[i] = (~frc).astype(np.float32); add[i] = val
    n = (128 * np.arange(4)[None, :, None] + np.arange(128)[:, None, None])
    j = np.arange(128)[None, None, :]
    ovl = ((16 * n < 64 * j + 64) & (16 * n + 32 > 64 * j) & (n <= 510)).astype(np.float32)
    i4b = (BIG * np.tile(np.eye(128, dtype=np.float32), (1, 4))).astype(ml_dtypes.bfloat16)
    return dict(cmask=cmask, smask=smask.astype(ml_dtypes.bfloat16), wmask=wmask.astype(ml_dtypes.bfloat16),
                keepm=keep, addm=add, ovl=np.ascontiguousarray(ovl), i4b=i4b)


_MASKS = {}


def seq_feat(resA, b, key):
    a = np.stack([resA[4 * b + sq][key] for sq in range(4)])
    rows = a.shape[1]
    a = a.reshape(4, rows, NT, 128).transpose(1, 2, 0, 3)
    return np.ascontiguousarray(a.reshape(rows, S))


def seq_tok(resA, b, key):
    a = np.stack([resA[4 * b + sq][key] for sq in range(4)])
    f = a.shape[2]
    a = a.reshape(4, NT, 128, f).transpose(1, 0, 2, 3)
    return np.ascontiguousarray(a.reshape(S, f))


def run_N(resA, cmp_pe_l, cmp_w1_l, cmp_w2_l):
    peT = np.ascontiguousarray(cmp_pe_l.transpose(2, 0, 1))
    w1 = np.ascontiguousarray(cmp_w1_l.reshape(2, 32, 64, 64).transpose(0, 2, 1, 3))
    maps = []
    seqs = {}
    for b in range(2):
        seqs[b] = (seq_feat(resA, b, "KTc"), seq_feat(resA, b, "KTs"), seq_tok(resA, b, "VV"))
    for c in range(NCORES):
        b, sq = c // 4, c % 4
        if sq not in _MASKS:
            _MASKS[sq] = nsa_masks(sq)
        m = dict(_MASKS[sq])
        m.update({"ident": IDENT, "qT": resA[c]["qT"], "KTc": seqs[b][0], "KTs": seqs[b][1], "VV": seqs[b][2],
                  "GL": np.ascontiguousarray(resA[c]["GZ"][:, 0:24]), "peT": peT, "w1": w1,
                  "w2": np.ascontiguousarray(cmp_w2_l)})
        maps.append(m)
    return _run("N", build_N, maps)


def recip(k, v):
    k.op("dve", lambda e: e.reciprocal(out=v.ap, in_=v.ap), [v], [v])


def rms_stat(k, src, n, junk, st):
    k.act(junk, src, AF.Square, accum=st[:, 0:1])
    k.ts("dve", st[:, 0:1], st[:, 0:1], 1.0 / n, EPS, op0=ALU.mult, op1=ALU.add)
    k.act(st[:, 0:1], st[:, 0:1], AF.Sqrt)
    recip(k, st[:, 0:1])


def ln_apply(k, dst, src, n, junk, st, gB, bB):
    k.act(junk, src, AF.Copy, accum=st[:, 0:1])
    k.act(junk, src, AF.Square, accum=st[:, 1:2])
    k.ts("dve", st[:, 0:1], st[:, 0:1], 1.0 / n, None, op0=ALU.mult)
    k.tt("dve", st[:, 2:3], st[:, 0:1], st[:, 0:1], ALU.mult)
    k.stt("dve", st[:, 1:2], st[:, 1:2], 1.0 / n, st[:, 2:3], ALU.mult, ALU.subtract)
    k.ts("dve", st[:, 1:2], st[:, 1:2], 1.0, EPS, op0=ALU.mult, op1=ALU.add)
    k.act(st[:, 1:2], st[:, 1:2], AF.Sqrt)
    recip(k, st[:, 1:2])
    k.ts("dve", dst, src, st[:, 0:1], st[:, 1:2], op0=ALU.subtract, op1=ALU.mult)
    k.tt("dve", dst, dst, gB, ALU.mult)
    k.tt("dve", dst, dst, bB, ALU.add)


def build_M():
    k = KB()
    x_d = k.dram("x", [NT, 128, D])
    yn_d = k.dram("yn", [TOK, 512])
    zg_d = k.dram("zg", [TOK, 512])
    hh_d = k.dram("hh", [128, 2, NT, 158])
    cw_d = k.dram("cw", [128, 2, 31]); cb_d = k.dram("cb", [128, 2])
    clg_d = k.dram("clg", [128, 256]); clb_d = k.dram("clb", [128, 256])
    glg_d = k.dram("glg", [128, 256]); glb_d = k.dram("glb", [128, 256])
    wsT_d = k.dram("wsT", [128, 4, 128]); triu_d = k.dram("triu", [128, 128]); bsT_d = k.dram("bsT", [128, 4])
    og_d = k.dram("ogcol", [128, 8]); wout_d = k.dram("w_out", [D, D]); ident_d = k.dram("ident", [128, 128])
    x1_d = k.dram("x1", [NT, 128, D], kind="ExternalOutput")

    ident = k.sb([128, 128]); k.dma("sp", ident[:, :], ident_d[:, :])
    hh = k.sb([128, 2, NT, 158], name="hh_sb"); k.dma("sp", hh[:, :, :, :], hh_d[:, :, :, :])
    cw = k.sb([128, 2, 31]); cb = k.sb([128, 2]); k.dma("sp", cw[:, :, :], cw_d[:, :, :]); k.dma("sp", cb[:, :], cb_d[:, :])
    clg = k.sb([128, 256]); clb = k.sb([128, 256]); glg = k.sb([128, 256]); glb = k.sb([128, 256])
    for t_, d_ in ((clg, clg_d), (clb, clb_d), (glg, glg_d), (glb, glb_d)):
        k.dma("sp", t_[:, :], d_[:, :])
    wsT = k.sb([128, 4, 128]); triu = k.sb([128, 128]); bsT = k.sb([128, 4]); ogcol = k.sb([128, 8])
    k.dma("sp", wsT[:, :, :], wsT_d[:, :, :]); k.dma("sp", triu[:, :], triu_d[:, :])
    k.dma("sp", bsT[:, :], bsT_d[:, :]); k.dma("sp", ogcol[:, :], og_d[:, :])
    wout = k.sb([128, 8, D], BF16, name="wout_sb")
    wstg = [k.sb([128, 8, 512]) for _ in range(2)]
    wv = wout_d.rearrange("(c p) n -> p c n", p=128)
    for h in range(2):
        k.dma("pool", wstg[h][:, :, :], wv[:, :, 512 * h:512 * h + 512])
        k.copy("pool", wout[:, :, 512 * h:512 * h + 512], wstg[h][:, :, :])
    k.tt("dve", wsT[:, :, :], wsT[:, :, :], bc3(triu, triu.t[:, :], 4, 1), ALU.mult)

    conv = k.sb([128, 2, NT, 128], name="conv_sb")
    for ch, en in ((0, "dve"), (1, "dve")):
        acc = conv.p(ch, (slice(None), ch, slice(None), slice(None)))
        for j in range(31):
            src = hh.p(ch, (slice(None), ch, slice(None), slice(j, j + 128)))
            if j == 0:
                k.ts(en, acc, src, cw[:, ch, 0:1], cb[:, ch:ch + 1], op0=ALU.mult, op1=ALU.add)
            else:
                k.stt(en, acc, src, cw[:, ch, j:j + 1], acc, ALU.mult, ALU.add)

    xtb = [k.sb([128, D]) for _ in range(2)]; ynb = [k.sb([128, 512]) for _ in range(2)]
    zgb = [k.sb([128, 512]) for _ in range(2)]; x1b = [k.sb([128, D]) for _ in range(2)]
    ycat = k.sb([128, D]); yT = k.sb([128, 8, 128], BF16); junk = k.sb([128, D]); st = k.sb([128, 4])
    cvs = k.sb([128, 256]); vln = k.sb([128, 256]); ygm = k.sb([128, 256])
    pcv = k.ps([128, 512]); pm = k.ps([128, 512]); pst = [k.ps([128, 512]) for _ in range(2)]
    po = [k.ps([128, 512]) for _ in range(2)]
    for i in range(NT):
        xt = xtb[i % 2]; ynt = ynb[i % 2]; zgt = zgb[i % 2]; x1t = x1b[i % 2]
        k.dma("sp", xt[:, :], x_d[i, :, :])
        k.dma("sp", ynt[:, :], yn_d[128 * i:128 * i + 128, :])
        k.dma("sp", zgt[:, :], zg_d[128 * i:128 * i + 128, :])
        for ch in range(2):
            k.tr(pcv[:, 128 * ch:128 * ch + 128], conv.p(ch, (slice(None), ch, i, slice(None))), ident[:, :])
        ln_apply(k, cvs[:, :], pcv[:, 0:256], 256, junk[:, 0:256], st, clg[:, :], clb[:, :])
        k.act(cvs[:, :], cvs[:, :], AF.Silu)
        rms_stat(k, cvs[:, :], 256, junk[:, 0:256], st)
        k.ts("dve", ycat[:, 768:1024], cvs[:, :], st[:, 0:1], None, op0=ALU.mult)
        k.act(zgt[:, :], zgt[:, :], AF.Gelu)
        ln_apply(k, vln[:, :], zgt[:, 256:512], 256, junk[:, 0:256], st, glg[:, :], glb[:, :])
        for g in range(4):
            k.mm(pm[:, 64 * g:64 * g + 64], wsT[:, g, :], vln[:, 64 * g:64 * g + 64])
        for g in range(4):
            k.stt("dve", ygm[:, 64 * g:64 * g + 64], pm[:, 64 * g:64 * g + 64], bsT[:, g:g + 1],
                  zgt[:, 64 * g:64 * g + 64], ALU.add, ALU.mult)
        rms_stat(k, ygm[:, :], 256, junk[:, 0:256], st)
        k.ts("dve", ycat[:, 512:768], ygm[:, :], st[:, 0:1], None, op0=ALU.mult)
        rms_stat(k, ynt[:, :], 512, junk[:, 0:512], st)
        k.ts("dve", ycat[:, 0:512], ynt[:, :], st[:, 0:1], None, op0=ALU.mult)
        for h in range(2):
            for j in range(4):
                c = 4 * h + j
                k.tr(pst[h][:, 128 * j:128 * j + 128], ycat[:, 128 * c:128 * c + 128], ident[:, :])
        for h in range(2):
            for j in range(4):
                c = 4 * h + j
                k.act(yT[:, c, :], pst[h][:, 128 * j:128 * j + 128], AF.Copy, scale=ogcol[:, c:c + 1])
        for h in range(2):
            for kc in range(8):
                k.mm(po[h][:, :], yT[:, kc, :], wout[:, kc, 512 * h:512 * h + 512], start=(kc == 0), stop=(kc == 7))
            k.tt("dve", x1t[:, 512 * h:512 * h + 512], xt[:, 512 * h:512 * h + 512], po[h][:, :], ALU.add)
        k.dma("pool", x1_d[i, :, :], x1t[:, :], is_output=True)
    return k.finish()


def bcast128(v):
    return np.ascontiguousarray(np.broadcast_to(v[None, :], (128, v.shape[0]))).astype(np.float32)


TRIU = np.triu(np.ones((128, 128), np.float32))


def run_M(x, resA, resN, P, l):
    maps = []
    hseq = {b: seq_feat(resA, b, "HT") for b in range(2)}
    cw = np.ascontiguousarray(P["conv_dw_w"][l].T.reshape(2, 128, 31).transpose(1, 0, 2))
    cb = np.ascontiguousarray(P["conv_dw_b"][l].reshape(2, 128).T)
    wsT = np.ascontiguousarray(P["gmlp_ws"][l].transpose(2, 0, 1))
    bsT = np.ascontiguousarray(P["gmlp_bs"][l].T)
    for c in range(NCORES):
        b, sq = c // 4, c % 4
        hp = np.concatenate([np.zeros((256, 30), np.float32), hseq[b]], axis=1)
        hh = np.zeros((128, 2, NT, 158), np.float32)
        for i in range(NT):
            ch = 4 * i + sq
            hh[:, :, i, :] = hp[:, 128 * ch:128 * ch + 158].reshape(2, 128, 158).transpose(1, 0, 2)
        maps.append({"x": np.ascontiguousarray(x[c]), "yn": resN[c]["YN"],
                     "zg": np.ascontiguousarray(resA[c]["GZ"][:, 24:536]), "hh": hh, "cw": cw, "cb": cb,
                     "clg": bcast128(P["conv_ln_g"][l]), "clb": bcast128(P["conv_ln_b"][l]),
                     "glg": bcast128(P["gmlp_ln_g"][l]), "glb": bcast128(P["gmlp_ln_b"][l]),
                     "wsT": wsT, "triu": TRIU, "bsT": bsT, "ogcol": gcol_of(P["mix_out_g"][l]),
                     "w_out": np.ascontiguousarray(P["w_out"][l]), "ident": IDENT})
    return _run("M", build_M, maps)


def build_X():
    k = KB()
    x_d = k.dram("x", [NT, 128, D]); mem_d = k.dram("mem", [2, 128, D])
    gx_d = k.dram("gx", [128, 8]); gm_d = k.dram("gm", [128, 8])
    wq_d = k.dram("wq", [D, D]); wk_d = k.dram("wk", [D, D]); wv_d = k.dram("wv", [D, D]); wo_d = k.dram("wo", [D, D])
    ident_d = k.dram("ident", [128, 128])
    x2_d = k.dram("x2", [NT, 128, D], kind="ExternalOutput")

    ident = k.sb([128, 128]); k.dma("sp", ident[:, :], ident_d[:, :])
    gx = k.sb([128, 8]); gm = k.sb([128, 8]); k.dma("sp", gx[:, :], gx_d[:, :]); k.dma("sp", gm[:, :], gm_d[:, :])
    ones = k.sb([128, 128], BF16); k.memset("dve", ones[:, :], 1.0)
    wq = k.sb([128, 8, D], BF16, name="wq_sb"); wo = k.sb([128, 8, D], BF16, name="wo_sb")
    wtmp = [k.sb([128, 8, 512]) for _ in range(2)]
    memnT = k.sb([128, 8, 256]); kT = k.sb([128, 8, 256], BF16); vv = k.sb([128, 2, D], BF16)
    xtb = [k.sb([128, D]) for _ in range(2)]; x2b = [k.sb([128, D]) for _ in range(2)]
    work = dict(junk=k.sb([128, D]), ssq=k.sb([128, 1]), rstd=k.sb([128, 1]), xs=k.sb([128, D]))
    hT = k.sb([128, 8, 128], BF16); qT = k.sb([128, 8, 128], BF16); attnT = k.sb([128, 8, 128], BF16)
    Pm = k.sb([128, 2, 128], BF16); rden = k.sb([128, 128])
    pst = [k.ps([128, 512]) for _ in range(2)]
    psc = k.ps([128, 512]); pden = k.ps([128, 512]); poT = k.ps([128, 512]); po = [k.ps([128, 512]) for _ in range(2)]

    def wview(wd):
        return wd.rearrange("(c p) n -> p c n", p=128)
    for j in range(2):
        xt = xtb[j]
        k.dma("sp", xt[:, :], mem_d[j, :, :])
        rms_transpose(k, xt[:, :], gm, ident, lambda c, tc: memnT[:, c, tc:tc + 128], 128 * j, work, pst)
    for h in range(2):
        k.dma("sp", wtmp[h][:, :, :], wview(wk_d)[:, :, 512 * h:512 * h + 512])
    for n_ in range(8):
        wb = wtmp[n_ // 4]
        for kc in range(8):
            k.mm(psc[:, 0:256], wb[:, kc, 128 * (n_ % 4):128 * (n_ % 4) + 128], memnT[:, kc, :], start=(kc == 0), stop=(kc == 7))
        k.copy("dve", kT[:, n_, :], psc[:, 0:256])
    for h in range(2):
        k.dma("sp", wtmp[h][:, :, :], wview(wv_d)[:, :, 512 * h:512 * h + 512])
    for mc in range(2):
        for h in range(2):
            for kc in range(8):
                k.mm(pden[:, :], memnT[:, kc, 128 * mc:128 * mc + 128], wtmp[h][:, kc, :], start=(kc == 0), stop=(kc == 7))
            k.copy("dve", vv[:, mc, 512 * h:512 * h + 512], pden[:, :])
    for wsrc, wdst in ((wq_d, wq), (wo_d, wo)):
        for h in range(2):
            k.dma("sp", wtmp[h][:, :, :], wview(wsrc)[:, :, 512 * h:512 * h + 512])
            k.copy("pool", wdst[:, :, 512 * h:512 * h + 512], wtmp[h][:, :, :])

    for i in range(NT):
        xt = xtb[i % 2]; x2t = x2b[i % 2]
        k.dma("sp", xt[:, :], x_d[i, :, :])
        rms_transpose(k, xt[:, :], gx, ident, lambda c, tc: hT[:, c, :], 0, work, pst)
        for h in range(2):
            for j in range(4):
                n_ = 4 * h + j
                for kc in range(8):
                    k.mm(pst[h][:, 128 * j:128 * j + 128], wq[:, kc, 128 * n_:128 * n_ + 128], hT[:, kc, :],
                         start=(kc == 0), stop=(kc == 7))
            k.act(V(qT.t[:, 4 * h:4 * h + 4, :].rearrange("p a b -> p (a b)"), qT.main, [], True), pst[h][:, :],
                  AF.Copy, scale=1.0 / 16.0)
        for hd in range(4):
            for mc in range(2):
                for dc in range(2):
                    k.mm(psc[:, 128 * mc:128 * mc + 128], kT[:, 2 * hd + dc, 128 * mc:128 * mc + 128], qT[:, 2 * hd + dc, :],
                         start=(dc == 0), stop=(dc == 1))
            k.act(V(Pm.t[:, :, :].rearrange("p a b -> p (a b)"), Pm.main, [], True), psc[:, 0:256], AF.Exp)
            for mc in range(2):
                k.mm(pden[:, 0:128], ones[:, :], Pm[:, mc, :], start=(mc == 0), stop=(mc == 1))
            k.op("dve", lambda e: e.reciprocal(out=rden.t[:, :], in_=pden.t[:, 0:128]), [pden[:, 0:128]], [rden[:, :]])
            for dc in range(2):
                d_ = 2 * hd + dc
                for mc in range(2):
                    k.mm(poT[:, 128 * dc:128 * dc + 128], vv[:, mc, 128 * d_:128 * d_ + 128], Pm[:, mc, :],
                         start=(mc == 0), stop=(mc == 1))
            for dc in range(2):
                k.tt("dve", attnT[:, 2 * hd + dc, :], poT[:, 128 * dc:128 * dc + 128], rden[:, :], ALU.mult)
        for h in range(2):
            for kc in range(8):
                k.mm(po[h][:, :], attnT[:, kc, :], wo[:, kc, 512 * h:512 * h + 512], start=(kc == 0), stop=(kc == 7))
            k.tt("dve", x2t[:, 512 * h:512 * h + 512], xt[:, 512 * h:512 * h + 512], po[h][:, :], ALU.add)
        k.dma("pool", x2_d[i, :, :], x2t[:, :], is_output=True)
    return k.finish()


def run_X(x1_own, P, l):
    maps = []
    for c in range(NCORES):
        b = c // 4
        maps.append({"x": x1_own[c], "mem": np.ascontiguousarray(P["mem"][b].reshape(2, 128, D)),
                     "gx": gcol_of(P["xattn_norm_g"][l]), "gm": gcol_of(P["mem_norm_g"][l]),
                     "wq": np.ascontiguousarray(P["xattn_wq"][l]), "wk": np.ascontiguousarray(P["xattn_wk"][l]),
                     "wv": np.ascontiguousarray(P["xattn_wv"][l]), "wo": np.ascontiguousarray(P["xattn_wo"][l]),
                     "ident": IDENT})
    return _run("X", build_X, maps)


NJ = DFF // 128


def build_F(final):
    MD = BF16
    TB = 4
    NB = NT // TB
    k = KB()
    x_d = k.dram("x", [NT, 128, D]); xh_d = k.dram("xh", [NB, 2 * TB, D])
    gf_d = k.dram("gf", [128, 8]); wup_d = k.dram("w_up", [D, 2 * DFF]); wdn_d = k.dram("w_down", [DFF, D])
    dw_d = k.dram("dw", [128, 2 * NJ, 3]); db_d = k.dram("db", [128, 2 * NJ])
    ident_d = k.dram("ident", [128, 128]); fg_d = k.dram("fg", [128, D])
    out_d = k.dram("xo", [NT, 128, D], kind="ExternalOutput")

    ident = k.sb([128, 128]); k.dma("sp", ident[:, :], ident_d[:, :])
    gf = k.sb([128, 8]); k.dma("sp", gf[:, :], gf_d[:, :])
    dw = k.sb([128, 2 * NJ, 3]); db = k.sb([128, 2 * NJ]); fg = k.sb([128, D])
    k.dma("sp", dw[:, :, :], dw_d[:, :, :]); k.dma("sp", db[:, :], db_d[:, :]); k.dma("sp", fg[:, :], fg_d[:, :])
    xtb = [k.sb([128, D]) for _ in range(6)]; xhb = k.sb([128, D]); xob = [k.sb([128, D]) for _ in range(2)]
    k.memset("dve", xhb[:, :], 0.0)
    work = dict(junk=k.sb([128, D]), ssq=k.sb([128, 1]), rstd=k.sb([128, 1]), xs=k.sb([128, D]))
    hT = k.sb([128, 8, 128 * TB], MD); hTh = k.sb([128, 8, 128], MD)
    gT = k.sb([128, NJ, 128 * TB], MD, name="gT_sb")
    wst = [k.sb([128, 2048]) for _ in range(2)]
    wub = [k.sb([128, 2, 8, 128], MD) for _ in range(2)]
    wd = k.sb([128, NJ, D], MD, name="wd_sb")
    ub = [k.sb([128, TB, 130]) for _ in range(2)]; hc = [k.sb([128, TB, 128]) for _ in range(2)]
    sgt = k.sb([128, TB, 128]); st = k.sb([128, 4])
    pst = [k.ps([128, 512]) for _ in range(2)]
    pgu = [k.ps([128, 512]) for _ in range(4)]
    ph = k.ps([128, 512])
    po = pst
    wuv = wup_d.rearrange("(c p) n -> p c n", p=128)
    wdv = wdn_d.rearrange("(j p) n -> p j n", p=128)
    nw = 0
    for j in range(NJ):
        ws = wst[nw % 2]; nw += 1
        k.dma("sp", ws[:, 0:D], wdv[:, j, :])
        k.copy("pool", wd[:, j, :], ws[:, 0:D])
    xcnt = 0
    for blk in range(NB):
        xts = []
        for u in range(TB):
            xt = xtb[xcnt % 6]; xcnt += 1
            xts.append(xt)
            k.dma("sp", xt[:, :], x_d[TB * blk + u, :, :])
            rms_transpose(k, xt[:, :], gf, ident, lambda c, tc: hT[:, c, tc:tc + 128], 128 * u, work, pst)
        k.dma("sp", xhb[0:2 * TB, :], xh_d[blk, :, :])
        rms_transpose(k, xhb[:, :], gf, ident, lambda c, tc: hTh[:, c, :], 0, work, pst)
        for j in range(NJ):
            ws = wst[nw % 2]; wu = wub[nw % 2]; nw += 1
            ws4 = V(ws.t[:, :].rearrange("p (s c n) -> p s c n", s=2, c=8), ws.main, [], True)
            k.dma("sp", V(ws.t[:, 0:1024].rearrange("p (c n) -> p c n", c=8), ws.main, [], True),
                  wuv[:, :, 128 * j:128 * j + 128])
            k.dma("pool", V(ws.t[:, 1024:2048].rearrange("p (c n) -> p c n", c=8), ws.main, [], True),
                  wuv[:, :, DFF + 128 * j:DFF + 128 * j + 128])
            k.copy("act", wu[:, 0, :, :], V(ws.t[:, 0:1024].rearrange("p (c n) -> p c n", c=8), ws.main, [], True))
            k.copy("pool", wu[:, 1, :, :], V(ws.t[:, 1024:2048].rearrange("p (c n) -> p c n", c=8), ws.main, [], True))
            for s_ in range(2):
                p = pgu[2 * (j % 2) + s_]
                for kc in range(8):
                    k.mm(p[:, :], wu[:, s_, kc, :], hT[:, kc, :], start=(kc == 0), stop=(kc == 7))
                for kc in range(8):
                    k.mm(ph[:, 8 * s_:8 * s_ + 8], wu[:, s_, kc, :], hTh[:, kc, 0:2 * TB], start=(kc == 0), stop=(kc == 7))
            for s_ in range(2):
                p = pgu[2 * (j % 2) + s_]
                u_ = ub[s_]
                k.act(u_[:, :, 2:130], v3(p, p.t[:, :], r=TB), AF.Copy)
                k.copy("dve", u_[:, :, 0:2], v3(ph, ph.t[:, 8 * s_:8 * s_ + 8], r=TB))
                cj = s_ * NJ + j
                h_ = hc[s_]
                k.ts("dve", h_[:, :, :], u_[:, :, 0:128], dw[:, cj, 0:1], db[:, cj:cj + 1], op0=ALU.mult, op1=ALU.add)
                k.stt("dve", h_[:, :, :], u_[:, :, 1:129], dw[:, cj, 1:2], h_[:, :, :], ALU.mult, ALU.add)
                k.stt("dve", h_[:, :, :], u_[:, :, 2:130], dw[:, cj, 2:3], h_[:, :, :], ALU.mult, ALU.add)
            k.act(sgt[:, :, :], hc[0][:, :, :], AF.Silu)
            k.tt("pool", v3(gT, gT.t[:, j, :], r=TB), sgt[:, :, :], hc[1][:, :, :], ALU.mult)
        for u in range(TB):
            xo = xob[u % 2]
            for h in range(2):
                for j in range(NJ):
                    k.mm(po[h][:, :], gT[:, j, 128 * u:128 * u + 128], wd[:, j, 512 * h:512 * h + 512],
                         start=(j == 0), stop=(j == NJ - 1))
                k.tt("dve", xo[:, 512 * h:512 * h + 512], xts[u][:, 512 * h:512 * h + 512], po[h][:, :], ALU.add)
            if final:
                rms_stat(k, xo[:, :], D, work["junk"][:, :], st)
                k.ts("dve", xo[:, :], xo[:, :], st[:, 0:1], None, op0=ALU.mult)
                k.tt("dve", xo[:, :], xo[:, :], fg[:, :], ALU.mult)
            k.dma("pool", out_d[TB * blk + u, :, :], xo[:, :], is_output=True)
    return k.finish()


def run_F(x2_own, P, l, final):
    maps = []
    xs = {}
    for b in range(2):
        a = np.stack([x2_own[4 * b + sq] for sq in range(4)])
        xs[b] = a.transpose(1, 0, 2, 3).reshape(S, D)
    dw = np.ascontiguousarray(P["ffn_dw_w"][l].T.reshape(2 * NJ, 128, 3).transpose(1, 0, 2))
    db = np.ascontiguousarray(P["ffn_dw_b"][l].reshape(2 * NJ, 128).T)
    for c in range(NCORES):
        b, sq = c // 4, c % 4
        xh = np.zeros((NT // 4, 8, D), np.float32)
        for i in range(NT):
            t0 = 128 * (4 * i + sq)
            if t0 >= 2:
                xh[i // 4, 2 * (i % 4):2 * (i % 4) + 2] = xs[b][t0 - 2:t0]
        maps.append({"x": x2_own[c], "xh": xh, "gf": gcol_of(P["ffn_norm_g"][l]),
                     "w_up": np.ascontiguousarray(P["ffn_w_up"][l]), "w_down": np.ascontiguousarray(P["ffn_w_down"][l]),
                     "dw": dw, "db": db, "ident": IDENT, "fg": bcast128(P["final_norm_g"])})
    name = "F1" if final else "F0"
    return _run(name, lambda: build_F(final), maps)


def kernel(**inputs):
    P = {k_: np.asarray(v, dtype=np.float32) for k_, v in inputs.items()}
    x_own = [own_tiles(P["x"], c) for c in range(NCORES)]
    for l in range(2):
        resA = run_A_own(x_own, P["w_in"][l], P["mix_norm_g"][l])
        resN = run_N(resA, P["cmp_pe"][l], P["cmp_w1"][l], P["cmp_w2"][l])
        resM = run_M(np.stack(x_own), resA, resN, P, l)
        resX = run_X([r["x1"] for r in resM], P, l)
        resF = run_F([r["x2"] for r in resX], P, l, final=(l == 1))
        x_own = [np.asarray(r["xo"]) for r in resF]
    out = np.zeros((2, S, D), np.float32)
    for c in range(NCORES):
        b, sq = c // 4, c % 4
        out[b].reshape(64, 128, D)[sq::4] = x_own[c]
    return out


def run_A_own(x_own, w_in_l, g_l):
    maps = []
    for c in range(NCORES):
        maps.append({"x": np.ascontiguousarray(x_own[c]), "w_in": np.ascontiguousarray(w_in_l), "gcol": gcol_of(g_l),
                     "ident": IDENT})
    return _run("A", build_A, maps)
```

```python
import numpy as np
from contextlib import ExitStack
import concourse.bass as bass
import concourse.mybir as mybir
from concourse.bass_utils import run_bass_kernel_spmd

F32 = mybir.dt.float32
BF16 = mybir.dt.bfloat16
AF = mybir.ActivationFunctionType
ALU = mybir.AluOpType
AX = mybir.AxisListType

NCORES = 8
D = 1024
S = 8192
NT = 16
TOK = NT * 128
IN_W = 2328
DFF = 2816
EPS = 1e-6
BIG = 30000.0
STOP_A = None
STOP_N = None


class Tr:
    __slots__ = ("w", "r")

    def __init__(self):
        self.w = {}
        self.r = {}


class V:
    __slots__ = ("ap", "main", "parts", "whole")

    def __init__(self, ap, main, parts, whole):
        self.ap, self.main, self.parts, self.whole = ap, main, parts, whole

    @property
    def trs(self):
        return [self.main] + list(self.parts)


class Buf:
    def __init__(self, t):
        self.t = t
        self.main = Tr()
        self.parts = {}

    def __getitem__(self, key):
        return V(self.t[key], self.main, list(self.parts.values()), True)

    def p(self, pk, key):
        if not isinstance(pk, (list, tuple)):
            pk = [pk]
        trs = []
        for q in pk:
            if q not in self.parts:
                self.parts[q] = Tr()
            trs.append(self.parts[q])
        return V(self.t[key], self.main, trs, False)


class Eng:
    def __init__(self, name, eng, sem, sid):
        self.name, self.eng, self.sem, self.sid = name, eng, sem, sid
        self.count = 0
        self.seen = {}


class KB:
    def __init__(self, ndma_sems=6):
        self.nc = bass.Bass("TRN2", target_bir_lowering=False)
        self.es = ExitStack()
        nc = self.nc
        self.sems = {}
        self.engs = {}
        for name, e in (("pe", nc.tensor), ("dve", nc.vector), ("act", nc.scalar),
                        ("pool", nc.gpsimd), ("sp", nc.sync)):
            sem = self.es.enter_context(nc.semaphore("s_" + name))
            sid = len(self.sems)
            self.sems[sid] = sem
            self.engs[name] = Eng(name, e, sem, sid)
        self.dq = {}
        for q in ("sp", "pool", "act"):
            lst = []
            for j in range(ndma_sems):
                sem = self.es.enter_context(nc.semaphore("d_%s%d" % (q, j)))
                sid = len(self.sems)
                self.sems[sid] = sem
                lst.append([sid, 0])
            self.dq[q] = [lst, 0]
        self.out_events = []
        self.nalloc = 0

    def sb(self, shape, dt=F32, name=None):
        self.nalloc += 1
        t = self.es.enter_context(self.nc.sbuf_tensor(name or "sb%d" % self.nalloc, list(shape), dt))
        return Buf(t)

    def ps(self, shape, dt=F32, name=None):
        self.nalloc += 1
        t = self.es.enter_context(self.nc.psum_tensor(name or "ps%d" % self.nalloc, list(shape), dt))
        return Buf(t)

    def dram(self, name, shape, dt=F32, kind="ExternalInput"):
        return self.nc.dram_tensor(name, list(shape), dt, kind=kind).ap()

    def _wait(self, E, deps):
        for sid, val in deps.items():
            if E.name == "pe" and sid == E.sid:
                continue
            if E.seen.get(sid, 0) >= val:
                continue
            E.eng.wait_ge(self.sems[sid], val)
            E.seen[sid] = val

    @staticmethod
    def _merge(d, o):
        for s, v in o.items():
            if d.get(s, 0) < v:
                d[s] = v

    def _deps(self, reads, writes):
        deps = {}
        for v in reads:
            if isinstance(v, V):
                for tr in v.trs:
                    self._merge(deps, tr.w)
        for v in writes:
            if isinstance(v, V):
                for tr in v.trs:
                    self._merge(deps, tr.w)
                    self._merge(deps, tr.r)
        return deps

    def _commit(self, ev, reads, writes):
        sid, val = ev
        for v in reads:
            if isinstance(v, V):
                for tr in ([v.main] if v.whole else v.parts):
                    if tr.r.get(sid, 0) < val:
                        tr.r[sid] = val
        for v in writes:
            if isinstance(v, V):
                if v.whole:
                    v.main.w = {sid: val}
                    v.main.r = {}
                    for tr in v.parts:
                        tr.w = {}
                        tr.r = {}
                else:
                    for tr in v.parts:
                        tr.w = {sid: val}
                        tr.r = {}

    def op(self, en, fn, reads, writes):
        E = self.engs[en]
        self._wait(E, self._deps(reads, writes))
        ins = fn(E.eng)
        E.count += 1
        ins.then_inc(E.sem, 1)
        self._commit((E.sid, E.count), reads, writes)

    @staticmethod
    def a(v):
        return v.ap if isinstance(v, V) else v

    def dma(self, q, out, in_, is_output=False, **kw):
        E = self.engs[q]
        lst, n = self.dq[q]
        slot = lst[n % len(lst)]
        self.dq[q][1] = n + 1
        if slot[1] > 0:
            self._wait(E, {slot[0]: slot[1]})
        self._wait(E, self._deps([in_], [out]))
        slot[1] += 16
        E.eng.dma_start(out=self.a(out), in_=self.a(in_), **kw).then_inc(self.sems[slot[0]], 16)
        ev = (slot[0], slot[1])
        self._commit(ev, [in_], [out])
        if is_output:
            self.out_events.append(ev)

    def mm(self, out, lhsT, rhs, start=True, stop=True):
        self.op("pe", lambda e: e.matmul(self.a(out), self.a(lhsT), self.a(rhs), start=start, stop=stop),
                [lhsT, rhs], [out])

    def tr(self, out, in_, ident):
        self.op("pe", lambda e: e.transpose(self.a(out), self.a(in_), self.a(ident)), [in_, ident], [out])

    def act(self, out, in_, func, bias=None, scale=None, accum=None, en="act"):
        kw = {}
        rd = [in_]
        if bias is not None:
            kw["bias"] = self.a(bias)
            rd.append(bias)
        if scale is not None:
            kw["scale"] = self.a(scale)
            rd.append(scale)
        wr = [out]
        if accum is not None:
            kw["accum_out"] = self.a(accum)
            wr.append(accum)
        self.op(en, lambda e: e.activation(out=self.a(out), in_=self.a(in_), func=func, **kw), rd, wr)

    def tt(self, en, out, a, b, op):
        self.op(en, lambda e: e.tensor_tensor(out=self.a(out), in0=self.a(a), in1=self.a(b), op=op), [a, b], [out])

    def ts(self, en, out, a, s1, s2=None, op0=ALU.mult, op1=None, accum=None):
        kw = {}
        if op1 is not None:
            kw["op1"] = op1
        wr = [out]
        if accum is not None:
            kw["accum_out"] = self.a(accum)
            wr.append(accum)
        self.op(en, lambda e: e.tensor_scalar(out=self.a(out), in0=self.a(a), scalar1=self.a(s1),
                                              scalar2=self.a(s2), op0=op0, **kw), [a, s1, s2], wr)

    def stt(self, en, out, a, s, b, op0, op1):
        self.op(en, lambda e: e.scalar_tensor_tensor(out=self.a(out), in0=self.a(a), scalar=self.a(s),
                                                     in1=self.a(b), op0=op0, op1=op1), [a, s, b], [out])

    def copy(self, en, out, in_):
        if en == "act":
            self.act(out, in_, AF.Copy)
        else:
            self.op(en, lambda e: e.tensor_copy(out=self.a(out), in_=self.a(in_)), [in_], [out])

    def memset(self, en, out, val):
        self.op(en, lambda e: e.memset(self.a(out), val), [], [out])

    def finish(self):
        E = self.engs["sp"]
        deps = {}
        for sid, val in self.out_events:
            if deps.get(sid, 0) < val:
                deps[sid] = val
        self._wait(E, deps)
        self.es.close()
        return self.nc


def rms_transpose(k, xt, gcol, ident, dstT, tcol, work, pst):
    junk, ssq, rstd, xs = work["junk"], work["ssq"], work["rstd"], work["xs"]
    k.act(junk[:, :], xt, AF.Square, accum=ssq[:, :])
    k.ts("dve", rstd[:, :], ssq[:, :], 1.0 / D, EPS, op0=ALU.mult, op1=ALU.add)
    k.act(rstd[:, :], rstd[:, :], AF.Sqrt)
    k.op("dve", lambda e: e.reciprocal(out=rstd.t[:, :], in_=rstd.t[:, :]), [rstd[:, :]], [rstd[:, :]])
    k.ts("dve", xs[:, :], xt, rstd[:, :], None, op0=ALU.mult)
    for h in range(2):
        for j in range(4):
            c = 4 * h + j
            k.tr(pst[h][:, 128 * j:128 * j + 128], xs[:, 128 * c:128 * c + 128], ident[:, :])
    for h in range(2):
        for j in range(4):
            c = 4 * h + j
            dv = dstT(c, tcol)
            k.act(dv, pst[h][:, 128 * j:128 * j + 128], AF.Copy, scale=gcol[:, c:c + 1])


def build_A():
    k = KB()
    x = k.dram("x", [NT, 128, D])
    w = k.dram("w_in", [D, IN_W])
    gcol_d = k.dram("gcol", [128, 8])
    ident_d = k.dram("ident", [128, 128])
    qT = k.dram("qT", [512, TOK], kind="ExternalOutput")
    KTc = k.dram("KTc", [256, TOK], kind="ExternalOutput")
    KTs = k.dram("KTs", [256, TOK], BF16, kind="ExternalOutput")
    VV = k.dram("VV", [TOK, 256], BF16, kind="ExternalOutput")
    GZ = k.dram("GZ", [TOK, 536], kind="ExternalOutput")
    HT = k.dram("HT", [256, TOK], kind="ExternalOutput")

    ident = k.sb([128, 128]); gcol = k.sb([128, 8])
    k.dma("sp", ident[:, :], ident_d[:, :])
    k.dma("sp", gcol[:, :], gcol_d[:, :])
    xnT = k.sb([128, 8, TOK], BF16, name="xnT")
    xts = [k.sb([128, D]) for _ in range(2)]
    work = dict(junk=k.sb([128, D]), ssq=k.sb([128, 1]), rstd=k.sb([128, 1]), xs=k.sb([128, D]))
    pst = [k.ps([128, 512]) for _ in range(2)]
    for i in range(NT):
        xt = xts[i % 2]
        k.dma("sp", xt[:, :], x[i, :, :])
        rms_transpose(k, xt[:, :], gcol, ident, lambda c, tc, i=i: xnT.p(i, (slice(None), c, slice(tc, tc + 128))),
                      128 * i, work, pst)

    wv = w.rearrange("(c p) n -> p c n", p=128)
    wbufs = [k.sb([128, 8, 512], BF16) for _ in range(2)]
    wstgA = [k.sb([128, 8, 512]) for _ in range(2)]
    pacc = [k.ps([128, 512]) for _ in range(4)]
    stg = [k.sb([128, 512]) for _ in range(3)]
    stgb = [k.sb([128, 512], BF16) for _ in range(3)]
    sg = k.sb([128, 512])
    cnt = {"w": 0, "p": 0, "s": 0}

    def loadw(c0, c1):
        wb = wbufs[cnt["w"] % 2]; ws = wstgA[cnt["w"] % 2]; cnt["w"] += 1
        k.dma("sp", ws[:, :, 0:c1 - c0], wv[:, :, c0:c1])
        k.copy("pool", wb[:, :, 0:c1 - c0], ws[:, :, 0:c1 - c0])
        return wb

    def nextp():
        p = pacc[cnt["p"] % 4]; cnt["p"] += 1
        return p

    def nexts(bf=False):
        s = (stgb if bf else stg)[cnt["s"] % 3]; cnt["s"] += 1
        return s

    def feat_block(wb, j0, tb):
        p = nextp()
        for kc in range(8):
            k.mm(p[:, :], wb[:, kc, j0:j0 + 128],
                 xnT.p([4 * tb + u for u in range(4)], (slice(None), kc, slice(512 * tb, 512 * tb + 512))),
                 start=(kc == 0), stop=(kc == 7))
        return p

    def tok_block(wb, j0, n, i):
        p = nextp()
        for kc in range(8):
            k.mm(p[:, 0:n], xnT.p(i, (slice(None), kc, slice(128 * i, 128 * i + 128))), wb[:, kc, j0:j0 + n],
                 start=(kc == 0), stop=(kc == 7))
        return p

    wb = loadw(0, 512)
    for j in range(4):
        for tb in range(4):
            p = feat_block(wb, 128 * j, tb)
            s = nexts()
            k.act(s[:, :], p[:, :], AF.Copy, scale=0.125)
            k.dma("pool", qT[128 * j:128 * j + 128, 512 * tb:512 * tb + 512], s[:, :], is_output=True)
    if STOP_A == "B1":
        return k.finish()
    wb = loadw(512, 1024)
    for j in range(3):
        for tb in range(4):
            p = feat_block(wb, 128 * j, tb)
            s = nexts(bf=(j == 2))
            k.copy("dve", s[:, :], p[:, :])
            dst = KTc[128 * j:128 * j + 128, 512 * tb:512 * tb + 512] if j < 2 else KTs[0:128, 512 * tb:512 * tb + 512]
            k.dma("pool" if j < 2 else "sp", dst, s[:, :], is_output=True)
    for i in range(NT):
        p = tok_block(wb, 384, 128, i)
        s = nexts(bf=True)
        k.copy("dve", s[:, 0:128], p[:, 0:128])
        k.dma("sp", VV[128 * i:128 * i + 128, 0:128], s[:, 0:128], is_output=True)
    if STOP_A == "B2":
        return k.finish()
    wb = loadw(1024, 1304)
    for tb in range(4):
        p = feat_block(wb, 0, tb)
        s = nexts(bf=True)
        k.copy("dve", s[:, :], p[:, :])
        k.dma("sp", KTs[128:256, 512 * tb:512 * tb + 512], s[:, :], is_output=True)
    if STOP_A == "B2a":
        return k.finish()
    for i in range(NT):
        if STOP_A == "B2b" and i == 1:
            return k.finish()
        p = tok_block(wb, 128, 152, i)
        s = nexts(bf=True)
        k.copy("dve", s[:, 0:128], p[:, 0:128])
        k.dma("sp", VV[128 * i:128 * i + 128, 128:256], s[:, 0:128], is_output=True)
        s2 = nexts()
        k.copy("dve", s2[:, 0:24], p[:, 128:152])
        k.dma("pool", GZ[128 * i:128 * i + 128, 0:24], s2[:, 0:24], is_output=True)
    if STOP_A == "B3":
        return k.finish()
    wb = loadw(1304, 1816)
    for i in range(NT):
        p = tok_block(wb, 0, 512, i)
        s = nexts()
        k.copy("dve", s[:, :], p[:, :])
        k.dma("pool", GZ[128 * i:128 * i + 128, 24:536], s[:, :], is_output=True)
    if STOP_A == "B4":
        return k.finish()
    wb = loadw(1816, 2328)
    for j in range(2):
        for tb in range(4):
            pa = feat_block(wb, 128 * j, tb)
            pg = feat_block(wb, 256 + 128 * j, tb)
            k.act(sg[:, :], pg[:, :], AF.Sigmoid)
            s = nexts()
            k.tt("dve", s[:, :], pa[:, :], sg[:, :], ALU.mult)
            k.dma("pool", HT[128 * j:128 * j + 128, 512 * tb:512 * tb + 512], s[:, :], is_output=True)
    return k.finish()


_PROGS = {}


def _prog(name, builder):
    if name not in _PROGS:
        _PROGS[name] = builder()
    return _PROGS[name]


def _run(name, builder, in_maps):
    nc = _prog(name, builder)
    res = run_bass_kernel_spmd(nc, in_maps, core_ids=list(range(NCORES)))
    return res.results


def own_tiles(a, core):
    b, sq = core // 4, core % 4
    t = a[b].reshape((64, 128) + a.shape[2:])
    return np.ascontiguousarray(t[sq::4])


def gcol_of(g):
    return np.ascontiguousarray(g.reshape(8, 128).T)


IDENT = np.eye(128, dtype=np.float32)


def run_A(x, w_in_l, g_l):
    maps = []
    for c in range(NCORES):
        maps.append({"x": own_tiles(x, c), "w_in": np.ascontiguousarray(w_in_l), "gcol": gcol_of(g_l),
                     "ident": IDENT})
    return _run("A", build_A, maps)


ND = BF16
TINY = 1e-30


def v3(buf, view2d_t, r=4):
    return V(view2d_t.rearrange("p (r t) -> p r t", r=r), buf.main, list(buf.parts.values()), True)


def bc3(buf, t2d, n, axis):
    if axis == 1:
        ap = t2d.unsqueeze(1).to_broadcast([t2d.shape[0], n, t2d.shape[1]])
    else:
        ap = t2d.unsqueeze(2).to_broadcast([t2d.shape[0], t2d.shape[1], n])
    return V(ap, buf.main, list(buf.parts.values()), True)


def build_N():
    k = KB()
    qT_d = k.dram("qT", [512, TOK])
    KTc_d = k.dram("KTc", [256, S])
    KTs_d = k.dram("KTs", [256, S], ND)
    VV_d = k.dram("VV", [S, 256], ND)
    GL_d = k.dram("GL", [TOK, 24])
    pe_d = k.dram("peT", [64, 2, 32])
    w1_d = k.dram("w1", [2, 64, 32, 64])
    w2_d = k.dram("w2", [2, 64, 64])
    cmask_d = k.dram("cmask", [NT, 4, 128, 128])
    smask_d = k.dram("smask", [128, 4, 128], ND)
    wmask_d = k.dram("wmask", [128, 8, 128], ND)
    keep_d = k.dram("keepm", [NT, 128, 128])
    add_d = k.dram("addm", [NT, 128, 128])
    ovl_d = k.dram("ovl", [128, 4, 128])
    i4_d = k.dram("i4b", [128, 512], ND)
    identN_d = k.dram("ident", [128, 128])
    YN = k.dram("YN", [TOK, 512], kind="ExternalOutput")

    ksT = k.sb([128, S], ND, "ksT"); kwT = k.sb([128, S], ND, "kwT")
    vs1 = k.sb([128, 64, 2, 65], ND, "vs1"); vw1 = k.sb([128, 64, 2, 65], ND, "vw1")
    kcv = k.sb([128, S], F32, "kcv")
    w1sb = k.sb([128, 32, 64]); peT = k.sb([64, 2, 32]); w2k = k.sb([64, 2, 128]); w2v = k.sb([64, 64])
    kcmpT = k.sb([128, 512]); hidT = k.sb([64, 512]); bias_sb = k.sb([64, 1])
    rhs_c = k.sb([128, 2, 4, 193])
    smask = k.sb([128, 4, 128], ND); wmask = k.sb([128, 8, 128], ND); i4b = k.sb([128, 512], ND)
    negX = k.sb([128, 128, 64], ND, "negX")
    Eb = [k.sb([128, 512]) for _ in range(4)]
    Pb = [k.sb([128, 512], ND) for _ in range(3)]
    cmb = [k.sb([128, 128]) for _ in range(2)]
    kpb = [k.sb([128, 128]) for _ in range(2)]; adb = [k.sb([128, 128]) for _ in range(2)]
    q32b = [k.sb([128, 4, 128]) for _ in range(2)]; qbb = [k.sb([128, 512], ND) for _ in range(2)]
    glb = [k.sb([128, 24]) for _ in range(2)]; sigb = [k.sb([128, 24]) for _ in range(2)]
    ynb = [k.sb([128, 512]) for _ in range(2)]
    imp = k.sb([128, 128]); imp2 = k.sb([128, 128]); wk = k.sb([128, 128]); m8 = k.sb([128, 16])
    den4 = k.sb([128, 4]); coef = k.sb([128, 4])
    pbig = [k.ps([128, 512]) for _ in range(3)]
    pc = k.ps([128, 2, 512]); pso = k.ps([128, 512]); poT = [k.ps([128, 512]) for _ in range(2)]
    psw = poT[0]
    oT_sb = k.sb([65, 512]); identN = k.sb([128, 128])
    k.dma("sp", identN[:, :], identN_d[:, :])
    cnt = {"p": 0, "P": 0, "cm": 0}

    def nextp():
        p = pbig[cnt["p"] % 3]; cnt["p"] += 1
        return p

    k.dma("sp", peT[:, :, :], pe_d[:, :, :])
    k.memset("pool", w2k[:, :, :], 0.0)
    k.dma("sp", w2k[:, 0, 0:64], w2_d[0, :, :])
    k.dma("sp", w2k[:, 1, 64:128], w2_d[0, :, :])
    k.dma("sp", w2v[:, :], w2_d[1, :, :])
    k.dma("sp", smask[:, :, :], smask_d[:, :, :])
    k.dma("sp", wmask[:, :, :], wmask_d[:, :, :])
    k.dma("sp", i4b[:, :], i4_d[:, :])
    k.memset("pool", rhs_c[:, :, :, 192:193], 1.0)
    for g in range(2):
        k.dma("sp", rhs_c[:, g, :, 0:128], ovl_d[:, :, :])
    k.memset("dve", kcmpT[:, :], 0.0)
    k.memset("dve", hidT[:, :], 0.0)

    if STOP_N == "const":
        return k.finish()
    for kv in range(2):
        for q4 in range(4):
            k.dma("sp", kcv[:, 2048 * q4:2048 * q4 + 2048], KTc_d[128 * kv:128 * kv + 128, 2048 * q4:2048 * q4 + 2048])
        for half in range(2):
            k.dma("pool", w1sb[64 * half:64 * half + 64, :, :], w1_d[kv, :, :, :])
        bp = pso
        for l in range(32):
            k.mm(bp[0:64, 0:1], w1sb[0:64, l, :], peT[:, kv, l:l + 1], start=(l == 0), stop=(l == 31))
        k.copy("dve", bias_sb[:, :], bp[0:64, 0:1])
        for g in range(2):
            hp = nextp()
            for l in range(32):
                k.mm(hp[0:64, 0:511], w1sb[64 * g:64 * g + 64, l, :], kcv[64 * g:64 * g + 64, l:l + 8161:16],
                     start=(l == 0), stop=(l == 31))
            k.act(hidT[:, 0:511], hp[0:64, 0:511], AF.Gelu, bias=bias_sb[:, 0:1])
            if kv == 0:
                op_ = nextp()
                k.mm(op_[:, 0:511], w2k[:, g, :], hidT[:, 0:511])
                k.copy("dve", kcmpT[64 * g:64 * g + 64, 0:511], op_[64 * g:64 * g + 64, 0:511])
            else:
                for cc in range(4):
                    k.mm(psw[:, 64 * cc:64 * cc + 64], hidT[:, 128 * cc:128 * cc + 128], w2v[:, :])
                for cc in range(4):
                    k.copy("dve", rhs_c[:, g, cc, 128:192], psw[:, 64 * cc:64 * cc + 64])

    if STOP_N == "cmp":
        return k.finish()
    for q4 in range(4):
        sl = slice(2048 * q4, 2048 * q4 + 2048)
        k.dma("sp", ksT[:, sl], KTs_d[0:128, sl])
    VVr = VV_d.rearrange("(c p) f -> p c f", p=128)
    k.memset("pool", vs1[:, :, :, 64:65], 1.0)
    k.memset("pool", vw1[:, :, :, 64:65], 1.0)
    for q4 in range(4):
        cs = slice(16 * q4, 16 * q4 + 16)
        for g in range(2):
            k.dma("sp", vs1[:, cs, g, 0:64], VVr[:, cs, 64 * g:64 * g + 64])
    for q4 in range(4):
        sl = slice(2048 * q4, 2048 * q4 + 2048)
        k.dma("sp", kwT[:, sl], KTs_d[128:256, sl])
    for q4 in range(4):
        cs = slice(16 * q4, 16 * q4 + 16)
        for g in range(2):
            k.dma("sp", vw1[:, cs, g, 0:64], VVr[:, cs, 128 + 64 * g:128 + 64 * g + 64])

    if STOP_N == "load":
        return k.finish()
    def untranspose(po):
        k.copy("dve", oT_sb[:, :], po[0:65, :])
        for r in range(4):
            k.tr(pso[:, 65 * r:65 * r + 65], oT_sb[:, 128 * r:128 * r + 128], identN[0:65, 0:65])

    def branch_out(ps_t, stride, g, gate_idx, sig, yn, first):
        for r in range(4):
            k.ts("dve", den4[:, r:r + 1], ps_t(r, 64, 65), TINY, None, op0=ALU.max)
        k.op("dve", lambda e: e.reciprocal(out=den4.t[:, :], in_=den4.t[:, :]), [den4[:, :]], [den4[:, :]])
        k.tt("dve", coef[:, :], sig[:, 12 * g + gate_idx:12 * g + 12:3], den4[:, :], ALU.mult)
        for r in range(4):
            h = 4 * g + r
            if first:
                k.ts("dve", yn[:, 64 * h:64 * h + 64], ps_t(r, 0, 64), coef[:, r:r + 1], None, op0=ALU.mult)
            else:
                k.stt("dve", yn[:, 64 * h:64 * h + 64], ps_t(r, 0, 64), coef[:, r:r + 1], yn[:, 64 * h:64 * h + 64],
                      ALU.mult, ALU.add)

    negXs = [negX, k.sb([128, 128, 64], ND, "negX1")]
    imps = [imp, k.sb([128, 128])]; imp2s = [imp2, k.sb([128, 128])]; wks = [wk, k.sb([128, 128])]
    m8s = [m8, k.sb([128, 16])]; den4c = [k.sb([128, 4]) for _ in range(2)]; coefc = [k.sb([128, 4]) for _ in range(2)]

    def bufs(i):
        return q32b[i % 2], qbb[i % 2], glb[i % 2], sigb[i % 2], ynb[i % 2], kpb[i % 2], adb[i % 2]

    def slot_loads(i):
        q32, qb, gl, sig, yn, kp, ad = bufs(i)
        for g in range(2):
            k.dma("sp", q32[64 * g:64 * g + 64, :, :],
                  qT_d[256 * g:256 * g + 256, 128 * i:128 * i + 128].rearrange("(r d) t -> d r t", d=64))
        k.dma("sp", gl[:, :], GL_d[128 * i:128 * i + 128, :])
        k.dma("sp", kp[:, :], keep_d[i, :, :])
        k.dma("sp", ad[:, :], add_d[i, :, :])
        q2d = V(q32.t[:, :, :].rearrange("p r t -> p (r t)"), q32.main, [], True)
        k.copy("dve", qb[:, :], q2d)
        k.act(sig[:, :], gl[:, :], AF.Sigmoid)

    def cmp_phase(i, g):
        q32, qb, gl, sig, yn, kp, ad = bufs(i)
        imp_, imp2_, wk_, m8_, d4, cf, nX = imps[g], imp2s[g], wks[g], m8s[g], den4c[g], coefc[g], negXs[g]
        ncc = (32 * i + 30) // 128 + 1
        gs = slice(64 * g, 64 * g + 64)
        q2g = V(q32.t[gs, :, :].rearrange("p r t -> p (r t)"), q32.main, [], True)
        for cc in range(ncc):
            cm = cmb[cnt["cm"] % 2]; cnt["cm"] += 1
            k.dma("pool", cm[:, :], cmask_d[i, cc, :, :])
            sc = nextp()
            k.mm(sc[:, :], kcmpT[gs, 128 * cc:128 * cc + 128], q2g)
            E = Eb[cc]
            k.act(E[:, :], sc[:, :], AF.Exp)
            k.tt("dve", v3(E, E.t[:, :]), v3(E, E.t[:, :]), bc3(cm, cm.t[:, :], 4, 1), ALU.mult)
        for r in range(4):
            for cc in range(ncc):
                k.mm(pc[:, r // 2, 193 * (r % 2):193 * (r % 2) + 193], Eb[cc][:, 128 * r:128 * r + 128],
                     rhs_c[:, g, cc, :], start=(cc == 0), stop=(cc == ncc - 1))
        for r in range(4):
            k.ts("dve", d4[:, r:r + 1], pc[:, r // 2, 193 * (r % 2) + 192:193 * (r % 2) + 193], TINY, None, op0=ALU.max)
        recip(k, d4[:, :])
        k.ts("dve", imp_[:, :], pc[:, 0, 0:128], d4[:, 0:1], None, op0=ALU.mult)
        for r in range(1, 4):
            k.stt("dve", imp_[:, :], pc[:, r // 2, 193 * (r % 2):193 * (r % 2) + 128], d4[:, r:r + 1], imp_[:, :],
                  ALU.mult, ALU.add)
        k.tt("dve", cf[:, :], sig[:, 12 * g:12 * g + 12:3], d4[:, :], ALU.mult)
        for r in range(4):
            h = 4 * g + r
            k.ts("dve", yn[:, 64 * h:64 * h + 64], pc[:, r // 2, 193 * (r % 2) + 128:193 * (r % 2) + 192],
                 cf[:, r:r + 1], None, op0=ALU.mult)
        k.tt("dve", imp2_[:, :], imp_[:, :], kp[:, :], ALU.mult)
        k.tt("dve", imp2_[:, :], imp2_[:, :], ad[:, :], ALU.add)
        k.op("dve", lambda e: e.max(out=m8_.t[:, 0:8], in_=imp2_.t[:, :]), [imp2_[:, :]], [m8_[:, :]])
        k.op("dve", lambda e: e.match_replace(out=wk_.t[:, :], in_to_replace=m8_.t[:, 0:8], in_values=imp2_.t[:, :],
                                              imm_value=-1e30), [m8_[:, :], imp2_[:, :]], [wk_[:, :]])
        k.op("dve", lambda e: e.max(out=m8_.t[:, 8:16], in_=wk_.t[:, :]), [wk_[:, :]], [m8_[:, :]])
        k.ts("dve", nX[:, :, :], bc3(imp2_, imp2_.t[:, :], 64, 2), m8_[:, 15:16], 1.0, op0=ALU.is_ge, op1=ALU.subtract)

    def attn_phase(i, g):
        q32, qb, gl, sig, yn, kp, ad = bufs(i)
        nX = negXs[g]
        gs = slice(64 * g, 64 * g + 64)
        nch = 4 * i + 4
        prev = None
        for c in range(nch):
            sp_ = nextp()
            k.mm(sp_[:, :], ksT[gs, 128 * c:128 * c + 128], qb[gs, :], start=True, stop=False)
            k.mm(sp_[:, :], V(nX.t[:, 2 * c:2 * c + 2, :].rearrange("p a b -> p (a b)"), nX.main, [], True),
                 i4b[:, :], start=False, stop=True)
            P = Pb[cnt["P"] % 3]; cnt["P"] += 1
            k.act(P[:, :], sp_[:, :], AF.Exp)
            if c >= 4 * i:
                k.tt("dve", v3(P, P.t[:, :]), v3(P, P.t[:, :]), bc3(smask, smask.t[:, c - 4 * i, :], 4, 1), ALU.mult)
            if prev is not None:
                k.mm(poT[0][0:65, :], vs1[:, prev[0], g, :], prev[1][:, :], start=(prev[0] == 0), stop=False)
            prev = (c, P)
        k.mm(poT[0][0:65, :], vs1[:, prev[0], g, :], prev[1][:, :], start=(prev[0] == 0), stop=True)
        untranspose(poT[0])
        branch_out(lambda r, a, b: pso[:, 65 * r + a:65 * r + b], 65, g, 1, sig, yn, False)
        cl = [c for c in range(4 * i - 4, 4 * i + 4) if c >= 0]
        prev = None
        for c in cl:
            rp = c - (4 * i - 4)
            sp_ = nextp()
            k.mm(sp_[:, :], kwT[gs, 128 * c:128 * c + 128], qb[gs, :])
            P = Pb[cnt["P"] % 3]; cnt["P"] += 1
            k.act(P[:, :], sp_[:, :], AF.Exp)
            k.tt("dve", v3(P, P.t[:, :]), v3(P, P.t[:, :]), bc3(wmask, wmask.t[:, rp, :], 4, 1), ALU.mult)
            if prev is not None:
                k.mm(poT[1][0:65, :], vw1[:, prev[0], g, :], prev[1][:, :], start=(prev[0] == cl[0]), stop=False)
            prev = (c, P)
        k.mm(poT[1][0:65, :], vw1[:, prev[0], g, :], prev[1][:, :], start=(prev[0] == cl[0]), stop=True)
        untranspose(poT[1])
        branch_out(lambda r, a, b: pso[:, 65 * r + a:65 * r + b], 65, g, 2, sig, yn, False)

    slot_loads(0)
    cmp_phase(0, 0)
    cmp_phase(0, 1)
    for i in range(NT):
        attn_phase(i, 0)
        if i + 1 < NT:
            slot_loads(i + 1)
            cmp_phase(i + 1, 0)
        attn_phase(i, 1)
        if i + 1 < NT:
            cmp_phase(i + 1, 1)
        k.dma("pool", YN[128 * i:128 * i + 128, :], ynb[i % 2][:, :], is_output=True)
    return k.finish()


def nsa_masks(sq):
    import ml_dtypes
    n_l = np.arange(128)[:, None]; tt = np.arange(128)[None, :]
    cmask = np.zeros((NT, 4, 128, 128), np.float32)
    for i in range(NT):
        bi = 4 * i + sq
        for cc in range(4):
            n = 128 * cc + n_l
            cmask[i, cc] = ((n <= 510) & (16 * n + 31 <= 128 * bi + tt)).astype(np.float32)
    kk = n_l
    smask = np.zeros((128, 4, 128), np.float32)
    for r in range(4):
        if r < sq:
            smask[:, r, :] = 1.0
        elif r == sq:
            smask[:, r, :] = (kk <= tt)
    wmask = np.zeros((128, 8, 128), np.float32)
    for rp in range(8):
        d = rp - sq
        if d == 0:
            wmask[:, rp, :] = (kk > tt)
        elif 1 <= d <= 3:
            wmask[:, rp, :] = 1.0
        elif d == 4:
            wmask[:, rp, :] = (kk <= tt)
    keep = np.ones((NT, 128, 128), np.float32); add = np.zeros((NT, 128, 128), np.float32)
    jj = np.arange(128)[None, :]
    for i in range(NT):
        bi = 4 * i + sq
        cur = (2 * bi + (np.arange(128) >= 64))[:, None]
        val = np.zeros((128, 128), np.float32); frc = np.zeros((128, 128), bool)
        m0 = np.broadcast_to(jj == 0, (128, 128)); val[m0] = 1e4; frc |= m0
        m1 = (jj == cur - 1); val[m1] = 1e4 + 1; frc |= m1
        m2 = (jj == cur); val[m2] = 1e4 + 2; frc |= m2
        fut = (jj > cur); val[fut] = -1e4; frc |= fut
        keep[i] = (~frc).astype(np.float32); add[i] = val
    n = (128 * np.arange(4)[None, :, None] + np.arange(128)[:, None, None])
    j = np.arange(128)[None, None, :]
    ovl = ((16 * n < 64 * j + 64) & (16 * n + 32 > 64 * j) & (n <= 510)).astype(np.float32)
    i4b = (BIG * np.tile(np.eye(128, dtype=np.float32), (1, 4))).astype(ml_dtypes.bfloat16)
    return dict(cmask=cmask, smask=smask.astype(ml_dtypes.bfloat16), wmask=wmask.astype(ml_dtypes.bfloat16),
                keepm=keep, addm=add, ovl=np.ascontiguousarray(ovl), i4b=i4b)


_MASKS = {}


def seq_feat(resA, b, key):
    a = np.stack([resA[4 * b + sq][key] for sq in range(4)])
    rows = a.shape[1]
    a = a.reshape(4, rows, NT, 128).transpose(1, 2, 0, 3)
    return np.ascontiguousarray(a.reshape(rows, S))


def seq_tok(resA, b, key):
    a = np.stack([resA[4 * b + sq][key] for sq in range(4)])
    f = a.shape[2]
    a = a.reshape(4, NT, 128, f).transpose(1, 0, 2, 3)
    return np.ascontiguousarray(a.reshape(S, f))


def run_N(resA, cmp_pe_l, cmp_w1_l, cmp_w2_l):
    peT = np.ascontiguousarray(cmp_pe_l.transpose(2, 0, 1))
    w1 = np.ascontiguousarray(cmp_w1_l.reshape(2, 32, 64, 64).transpose(0, 2, 1, 3))
    maps = []
    seqs = {}
    for b in range(2):
        seqs[b] = (seq_feat(resA, b, "KTc"), seq_feat(resA, b, "KTs"), seq_tok(resA, b, "VV"))
    for c in range(NCORES):
        b, sq = c // 4, c % 4
        if sq not in _MASKS:
            _MASKS[sq] = nsa_masks(sq)
        m = dict(_MASKS[sq])
        m.update({"ident": IDENT, "qT": resA[c]["qT"], "KTc": seqs[b][0], "KTs": seqs[b][1], "VV": seqs[b][2],
                  "GL": np.ascontiguousarray(resA[c]["GZ"][:, 0:24]), "peT": peT, "w1": w1,
                  "w2": np.ascontiguousarray(cmp_w2_l)})
        maps.append(m)
    return _run("N", build_N, maps)


def recip(k, v):
    k.op("dve", lambda e: e.reciprocal(out=v.ap, in_=v.ap), [v], [v])


def rms_stat(k, src, n, junk, st):
    k.act(junk, src, AF.Square, accum=st[:, 0:1])
    k.ts("dve", st[:, 0:1], st[:, 0:1], 1.0 / n, EPS, op0=ALU.mult, op1=ALU.add)
    k.act(st[:, 0:1], st[:, 0:1], AF.Sqrt)
    recip(k, st[:, 0:1])


def ln_apply(k, dst, src, n, junk, st, gB, bB):
    k.act(junk, src, AF.Copy, accum=st[:, 0:1])
    k.act(junk, src, AF.Square, accum=st[:, 1:2])
    k.ts("dve", st[:, 0:1], st[:, 0:1], 1.0 / n, None, op0=ALU.mult)
    k.tt("dve", st[:, 2:3], st[:, 0:1], st[:, 0:1], ALU.mult)
    k.stt("dve", st[:, 1:2], st[:, 1:2], 1.0 / n, st[:, 2:3], ALU.mult, ALU.subtract)
    k.ts("dve", st[:, 1:2], st[:, 1:2], 1.0, EPS, op0=ALU.mult, op1=ALU.add)
    k.act(st[:, 1:2], st[:, 1:2], AF.Sqrt)
    recip(k, st[:, 1:2])
    k.ts("dve", dst, src, st[:, 0:1], st[:, 1:2], op0=ALU.subtract, op1=ALU.mult)
    k.tt("dve", dst, dst, gB, ALU.mult)
    k.tt("dve", dst, dst, bB, ALU.add)


def build_M():
    k = KB()
    x_d = k.dram("x", [NT, 128, D])
    yn_d = k.dram("yn", [TOK, 512])
    zg_d = k.dram("zg", [TOK, 512])
    hh_d = k.dram("hh", [128, 2, NT, 158])
    cw_d = k.dram("cw", [128, 2, 31]); cb_d = k.dram("cb", [128, 2])
    clg_d = k.dram("clg", [128, 256]); clb_d = k.dram("clb", [128, 256])
    glg_d = k.dram("glg", [128, 256]); glb_d = k.dram("glb", [128, 256])
    wsT_d = k.dram("wsT", [128, 4, 128]); triu_d = k.dram("triu", [128, 128]); bsT_d = k.dram("bsT", [128, 4])
    og_d = k.dram("ogcol", [128, 8]); wout_d = k.dram("w_out", [D, D]); ident_d = k.dram("ident", [128, 128])
    x1_d = k.dram("x1", [NT, 128, D], kind="ExternalOutput")

    ident = k.sb([128, 128]); k.dma("sp", ident[:, :], ident_d[:, :])
    hh = k.sb([128, 2, NT, 158], name="hh_sb"); k.dma("sp", hh[:, :, :, :], hh_d[:, :, :, :])
    cw = k.sb([128, 2, 31]); cb = k.sb([128, 2]); k.dma("sp", cw[:, :, :], cw_d[:, :, :]); k.dma("sp", cb[:, :], cb_d[:, :])
    clg = k.sb([128, 256]); clb = k.sb([128, 256]); glg = k.sb([128, 256]); glb = k.sb([128, 256])
    for t_, d_ in ((clg, clg_d), (clb, clb_d), (glg, glg_d), (glb, glb_d)):
        k.dma("sp", t_[:, :], d_[:, :])
    wsT = k.sb([128, 4, 128]); triu = k.sb([128, 128]); bsT = k.sb([128, 4]); ogcol = k.sb([128, 8])
    k.dma("sp", wsT[:, :, :], wsT_d[:, :, :]); k.dma("sp", triu[:, :], triu_d[:, :])
    k.dma("sp", bsT[:, :], bsT_d[:, :]); k.dma("sp", ogcol[:, :], og_d[:, :])
    wout = k.sb([128, 8, D], BF16, name="wout_sb")
    wstg = [k.sb([128, 8, 512]) for _ in range(2)]
    wv = wout_d.rearrange("(c p) n -> p c n", p=128)
    for h in range(2):
        k.dma("pool", wstg[h][:, :, :], wv[:, :, 512 * h:512 * h + 512])
        k.copy("pool", wout[:, :, 512 * h:512 * h + 512], wstg[h][:, :, :])
    k.tt("dve", wsT[:, :, :], wsT[:, :, :], bc3(triu, triu.t[:, :], 4, 1), ALU.mult)

    conv = k.sb([128, 2, NT, 128], name="conv_sb")
    for ch, en in ((0, "dve"), (1, "dve")):
        acc = conv.p(ch, (slice(None), ch, slice(None), slice(None)))
        for j in range(31):
            src = hh.p(ch, (slice(None), ch, slice(None), slice(j, j + 128)))
            if j == 0:
                k.ts(en, acc, src, cw[:, ch, 0:1], cb[:, ch:ch + 1], op0=ALU.mult, op1=ALU.add)
            else:
                k.stt(en, acc, src, cw[:, ch, j:j + 1], acc, ALU.mult, ALU.add)

    xtb = [k.sb([128, D]) for _ in range(2)]; ynb = [k.sb([128, 512]) for _ in range(2)]
    zgb = [k.sb([128, 512]) for _ in range(2)]; x1b = [k.sb([128, D]) for _ in range(2)]
    ycat = k.sb([128, D]); yT = k.sb([128, 8, 128], BF16); junk = k.sb([128, D]); st = k.sb([128, 4])
    cvs = k.sb([128, 256]); vln = k.sb([128, 256]); ygm = k.sb([128, 256])
    pcv = k.ps([128, 512]); pm = k.ps([128, 512]); pst = [k.ps([128, 512]) for _ in range(2)]
    po = [k.ps([128, 512]) for _ in range(2)]
    for i in range(NT):
        xt = xtb[i % 2]; ynt = ynb[i % 2]; zgt = zgb[i % 2]; x1t = x1b[i % 2]
        k.dma("sp", xt[:, :], x_d[i, :, :])
        k.dma("sp", ynt[:, :], yn_d[128 * i:128 * i + 128, :])
        k.dma("sp", zgt[:, :], zg_d[128 * i:128 * i + 128, :])
        for ch in range(2):
            k.tr(pcv[:, 128 * ch:128 * ch + 128], conv.p(ch, (slice(None), ch, i, slice(None))), ident[:, :])
        ln_apply(k, cvs[:, :], pcv[:, 0:256], 256, junk[:, 0:256], st, clg[:, :], clb[:, :])
        k.act(cvs[:, :], cvs[:, :], AF.Silu)
        rms_stat(k, cvs[:, :], 256, junk[:, 0:256], st)
        k.ts("dve", ycat[:, 768:1024], cvs[:, :], st[:, 0:1], None, op0=ALU.mult)
        k.act(zgt[:, :], zgt[:, :], AF.Gelu)
        ln_apply(k, vln[:, :], zgt[:, 256:512], 256, junk[:, 0:256], st, glg[:, :], glb[:, :])
        for g in range(4):
            k.mm(pm[:, 64 * g:64 * g + 64], wsT[:, g, :], vln[:, 64 * g:64 * g + 64])
        for g in range(4):
            k.stt("dve", ygm[:, 64 * g:64 * g + 64], pm[:, 64 * g:64 * g + 64], bsT[:, g:g + 1],
                  zgt[:, 64 * g:64 * g + 64], ALU.add, ALU.mult)
        rms_stat(k, ygm[:, :], 256, junk[:, 0:256], st)
        k.ts("dve", ycat[:, 512:768], ygm[:, :], st[:, 0:1], None, op0=ALU.mult)
        rms_stat(k, ynt[:, :], 512, junk[:, 0:512], st)
        k.ts("dve", ycat[:, 0:512], ynt[:, :], st[:, 0:1], None, op0=ALU.mult)
        for h in range(2):
            for j in range(4):
                c = 4 * h + j
                k.tr(pst[h][:, 128 * j:128 * j + 128], ycat[:, 128 * c:128 * c + 128], ident[:, :])
        for h in range(2):
            for j in range(4):
                c = 4 * h + j
                k.act(yT[:, c, :], pst[h][:, 128 * j:128 * j + 128], AF.Copy, scale=ogcol[:, c:c + 1])
        for h in range(2):
            for kc in range(8):
                k.mm(po[h][:, :], yT[:, kc, :], wout[:, kc, 512 * h:512 * h + 512], start=(kc == 0), stop=(kc == 7))
            k.tt("dve", x1t[:, 512 * h:512 * h + 512], xt[:, 512 * h:512 * h + 512], po[h][:, :], ALU.add)
        k.dma("pool", x1_d[i, :, :], x1t[:, :], is_output=True)
    return k.finish()


def bcast128(v):
    return np.ascontiguousarray(np.broadcast_to(v[None, :], (128, v.shape[0]))).astype(np.float32)


TRIU = np.triu(np.ones((128, 128), np.float32))


def run_M(x, resA, resN, P, l):
    maps = []
    hseq = {b: seq_feat(resA, b, "HT") for b in range(2)}
    cw = np.ascontiguousarray(P["conv_dw_w"][l].T.reshape(2, 128, 31).transpose(1, 0, 2))
    cb = np.ascontiguousarray(P["conv_dw_b"][l].reshape(2, 128).T)
    wsT = np.ascontiguousarray(P["gmlp_ws"][l].transpose(2, 0, 1))
    bsT = np.ascontiguousarray(P["gmlp_bs"][l].T)
    for c in range(NCORES):
        b, sq = c // 4, c % 4
        hp = np.concatenate([np.zeros((256, 30), np.float32), hseq[b]], axis=1)
        hh = np.zeros((128, 2, NT, 158), np.float32)
        for i in range(NT):
            ch = 4 * i + sq
            hh[:, :, i, :] = hp[:, 128 * ch:128 * ch + 158].reshape(2, 128, 158).transpose(1, 0, 2)
        maps.append({"x": np.ascontiguousarray(x[c]), "yn": resN[c]["YN"],
                     "zg": np.ascontiguousarray(resA[c]["GZ"][:, 24:536]), "hh": hh, "cw": cw, "cb": cb,
                     "clg": bcast128(P["conv_ln_g"][l]), "clb": bcast128(P["conv_ln_b"][l]),
                     "glg": bcast128(P["gmlp_ln_g"][l]), "glb": bcast128(P["gmlp_ln_b"][l]),
                     "wsT": wsT, "triu": TRIU, "bsT": bsT, "ogcol": gcol_of(P["mix_out_g"][l]),
                     "w_out": np.ascontiguousarray(P["w_out"][l]), "ident": IDENT})
    return _run("M", build_M, maps)


def build_X():
    k = KB()
    x_d = k.dram("x", [NT, 128, D]); mem_d = k.dram("mem", [2, 128, D])
    gx_d = k.dram("gx", [128, 8]); gm_d = k.dram("gm", [128, 8])
    wq_d = k.dram("wq", [D, D]); wk_d = k.dram("wk", [D, D]); wv_d = k.dram("wv", [D, D]); wo_d = k.dram("wo", [D, D])
    ident_d = k.dram("ident", [128, 128])
    x2_d = k.dram("x2", [NT, 128, D], kind="ExternalOutput")

    ident = k.sb([128, 128]); k.dma("sp", ident[:, :], ident_d[:, :])
    gx = k.sb([128, 8]); gm = k.sb([128, 8]); k.dma("sp", gx[:, :], gx_d[:, :]); k.dma("sp", gm[:, :], gm_d[:, :])
    ones = k.sb([128, 128], BF16); k.memset("dve", ones[:, :], 1.0)
    wq = k.sb([128, 8, D], BF16, name="wq_sb"); wo = k.sb([128, 8, D], BF16, name="wo_sb")
    wtmp = [k.sb([128, 8, 512]) for _ in range(2)]
    memnT = k.sb([128, 8, 256]); kT = k.sb([128, 8, 256], BF16); vv = k.sb([128, 2, D], BF16)
    xtb = [k.sb([128, D]) for _ in range(2)]; x2b = [k.sb([128, D]) for _ in range(2)]
    work = dict(junk=k.sb([128, D]), ssq=k.sb([128, 1]), rstd=k.sb([128, 1]), xs=k.sb([128, D]))
    hT = k.sb([128, 8, 128], BF16); qT = k.sb([128, 8, 128], BF16); attnT = k.sb([128, 8, 128], BF16)
    Pm = k.sb([128, 2, 128], BF16); rden = k.sb([128, 128])
    pst = [k.ps([128, 512]) for _ in range(2)]
    psc = k.ps([128, 512]); pden = k.ps([128, 512]); poT = k.ps([128, 512]); po = [k.ps([128, 512]) for _ in range(2)]

    def wview(wd):
        return wd.rearrange("(c p) n -> p c n", p=128)
    for j in range(2):
        xt = xtb[j]
        k.dma("sp", xt[:, :], mem_d[j, :, :])
        rms_transpose(k, xt[:, :], gm, ident, lambda c, tc: memnT[:, c, tc:tc + 128], 128 * j, work, pst)
    for h in range(2):
        k.dma("sp", wtmp[h][:, :, :], wview(wk_d)[:, :, 512 * h:512 * h + 512])
    for n_ in range(8):
        wb = wtmp[n_ // 4]
        for kc in range(8):
            k.mm(psc[:, 0:256], wb[:, kc, 128 * (n_ % 4):128 * (n_ % 4) + 128], memnT[:, kc, :], start=(kc == 0), stop=(kc == 7))
        k.copy("dve", kT[:, n_, :], psc[:, 0:256])
    for h in range(2):
        k.dma("sp", wtmp[h][:, :, :], wview(wv_d)[:, :, 512 * h:512 * h + 512])
    for mc in range(2):
        for h in range(2):
            for kc in range(8):
                k.mm(pden[:, :], memnT[:, kc, 128 * mc:128 * mc + 128], wtmp[h][:, kc, :], start=(kc == 0), stop=(kc == 7))
            k.copy("dve", vv[:, mc, 512 * h:512 * h + 512], pden[:, :])
    for wsrc, wdst in ((wq_d, wq), (wo_d, wo)):
        for h in range(2):
            k.dma("sp", wtmp[h][:, :, :], wview(wsrc)[:, :, 512 * h:512 * h + 512])
            k.copy("pool", wdst[:, :, 512 * h:512 * h + 512], wtmp[h][:, :, :])

    for i in range(NT):
        xt = xtb[i % 2]; x2t = x2b[i % 2]
        k.dma("sp", xt[:, :], x_d[i, :, :])
        rms_transpose(k, xt[:, :], gx, ident, lambda c, tc: hT[:, c, :], 0, work, pst)
        for h in range(2):
            for j in range(4):
                n_ = 4 * h + j
                for kc in range(8):
                    k.mm(pst[h][:, 128 * j:128 * j + 128], wq[:, kc, 128 * n_:128 * n_ + 128], hT[:, kc, :],
                         start=(kc == 0), stop=(kc == 7))
            k.act(V(qT.t[:, 4 * h:4 * h + 4, :].rearrange("p a b -> p (a b)"), qT.main, [], True), pst[h][:, :],
                  AF.Copy, scale=1.0 / 16.0)
        for hd in range(4):
            for mc in range(2):
                for dc in range(2):
                    k.mm(psc[:, 128 * mc:128 * mc + 128], kT[:, 2 * hd + dc, 128 * mc:128 * mc + 128], qT[:, 2 * hd + dc, :],
                         start=(dc == 0), stop=(dc == 1))
            k.act(V(Pm.t[:, :, :].rearrange("p a b -> p (a b)"), Pm.main, [], True), psc[:, 0:256], AF.Exp)
            for mc in range(2):
                k.mm(pden[:, 0:128], ones[:, :], Pm[:, mc, :], start=(mc == 0), stop=(mc == 1))
            k.op("dve", lambda e: e.reciprocal(out=rden.t[:, :], in_=pden.t[:, 0:128]), [pden[:, 0:128]], [rden[:, :]])
            for dc in range(2):
                d_ = 2 * hd + dc
                for mc in range(2):
                    k.mm(poT[:, 128 * dc:128 * dc + 128], vv[:, mc, 128 * d_:128 * d_ + 128], Pm[:, mc, :],
                         start=(mc == 0), stop=(mc == 1))
            for dc in range(2):
                k.tt("dve", attnT[:, 2 * hd + dc, :], poT[:, 128 * dc:128 * dc + 128], rden[:, :], ALU.mult)
        for h in range(2):
            for kc in range(8):
                k.mm(po[h][:, :], attnT[:, kc, :], wo[:, kc, 512 * h:512 * h + 512], start=(kc == 0), stop=(kc == 7))
            k.tt("dve", x2t[:, 512 * h:512 * h + 512], xt[:, 512 * h:512 * h + 512], po[h][:, :], ALU.add)
        k.dma("pool", x2_d[i, :, :], x2t[:, :], is_output=True)
    return k.finish()


def run_X(x1_own, P, l):
    maps = []
    for c in range(NCORES):
        b = c // 4
        maps.append({"x": x1_own[c], "mem": np.ascontiguousarray(P["mem"][b].reshape(2, 128, D)),
                     "gx": gcol_of(P["xattn_norm_g"][l]), "gm": gcol_of(P["mem_norm_g"][l]),
                     "wq": np.ascontiguousarray(P["xattn_wq"][l]), "wk": np.ascontiguousarray(P["xattn_wk"][l]),
                     "wv": np.ascontiguousarray(P["xattn_wv"][l]), "wo": np.ascontiguousarray(P["xattn_wo"][l]),
                     "ident": IDENT})
    return _run("X", build_X, maps)


NJ = DFF // 128


def build_F(final):
    MD = BF16
    TB = 4
    NB = NT // TB
    k = KB()
    x_d = k.dram("x", [NT, 128, D]); xh_d = k.dram("xh", [NB, 2 * TB, D])
    gf_d = k.dram("gf", [128, 8]); wup_d = k.dram("w_up", [D, 2 * DFF]); wdn_d = k.dram("w_down", [DFF, D])
    dw_d = k.dram("dw", [128, 2 * NJ, 3]); db_d = k.dram("db", [128, 2 * NJ])
    ident_d = k.dram("ident", [128, 128]); fg_d = k.dram("fg", [128, D])
    out_d = k.dram("xo", [NT, 128, D], kind="ExternalOutput")

    ident = k.sb([128, 128]); k.dma("sp", ident[:, :], ident_d[:, :])
    gf = k.sb([128, 8]); k.dma("sp", gf[:, :], gf_d[:, :])
    dw = k.sb([128, 2 * NJ, 3]); db = k.sb([128, 2 * NJ]); fg = k.sb([128, D])
    k.dma("sp", dw[:, :, :], dw_d[:, :, :]); k.dma("sp", db[:, :], db_d[:, :]); k.dma("sp", fg[:, :], fg_d[:, :])
    xtb = [k.sb([128, D]) for _ in range(6)]; xhb = k.sb([128, D]); xob = [k.sb([128, D]) for _ in range(2)]
    k.memset("dve", xhb[:, :], 0.0)
    work = dict(junk=k.sb([128, D]), ssq=k.sb([128, 1]), rstd=k.sb([128, 1]), xs=k.sb([128, D]))
    hT = k.sb([128, 8, 128 * TB], MD); hTh = k.sb([128, 8, 128], MD)
    gT = k.sb([128, NJ, 128 * TB], MD, name="gT_sb")
    wst = [k.sb([128, 2, 8, 512]) for _ in range(2)]
    wub = [k.sb([128, 2, 8, 128], MD) for _ in range(2)]
    wd = k.sb([128, NJ, D], MD, name="wd_sb")
    ub = [k.sb([128, TB, 130]) for _ in range(2)]; hc = [k.sb([128, TB, 128]) for _ in range(2)]
    sgt = k.sb([128, TB, 128]); st = k.sb([128, 4])
    pst = [k.ps([128, 512]) for _ in range(2)]
    pgu = [k.ps([128, 512]) for _ in range(2)]
    ph = k.ps([128, 512])
    po = [k.ps([128, 512]) for _ in range(2)]
    wuv = wup_d.rearrange("(c p) n -> p c n", p=128)
    wdv = wdn_d.rearrange("(j p) n -> p j n", p=128)
    nw = 0
    for j in range(NJ):
        ws = wst[nw % 2]; nw += 1
        wsv = V(ws.t[:, 0, 0:2, :].rearrange("p a b -> p (a b)"), ws.main, [], True)
        k.dma("sp" if j % 2 == 0 else "pool", wsv, wdv[:, j, :])
        k.copy("pool" if j % 2 == 0 else "dve", wd[:, j, :], wsv)
    xcnt = 0
    for blk in range(NB):
        xts = []
        for u in range(TB):
            xt = xtb[xcnt % 6]; xcnt += 1
            xts.append(xt)
            k.dma("sp", xt[:, :], x_d[TB * blk + u, :, :])
            rms_transpose(k, xt[:, :], gf, ident, lambda c, tc: hT[:, c, tc:tc + 128], 128 * u, work, pst)
        k.dma("sp", xhb[0:2 * TB, :], xh_d[blk, :, :])
        rms_transpose(k, xhb[:, :], gf, ident, lambda c, tc: hTh[:, c, :], 0, work, pst)
        for j in range(NJ):
            if j % 4 == 0:
                ws = wst[nw % 2]; nw += 1
                ncol = 128 * min(4, NJ - j)
                k.dma("sp", ws[:, 0, :, 0:ncol], wuv[:, :, 128 * j:128 * j + ncol])
                k.dma("pool", ws[:, 1, :, 0:ncol], wuv[:, :, DFF + 128 * j:DFF + 128 * j + ncol])
            jj = j % 4
            wu = wub[j % 2]
            k.copy("act", wu[:, 0, :, :], ws[:, 0, :, 128 * jj:128 * jj + 128])
            k.copy("pool", wu[:, 1, :, :], ws[:, 1, :, 128 * jj:128 * jj + 128])
            for s_ in range(2):
                p = pgu[s_]
                for kc in range(8):
                    k.mm(p[:, :], wu[:, s_, kc, :], hT[:, kc, :], start=(kc == 0), stop=(kc == 7))
                for kc in range(8):
                    k.mm(ph[:, 8 * s_:8 * s_ + 8], wu[:, s_, kc, :], hTh[:, kc, 0:2 * TB], start=(kc == 0), stop=(kc == 7))
            for s_ in range(2):
                p = pgu[s_]
                u_ = ub[s_]
                k.act(u_[:, :, 2:130], v3(p, p.t[:, :], r=TB), AF.Copy)
                k.copy("dve", u_[:, :, 0:2], v3(ph, ph.t[:, 8 * s_:8 * s_ + 8], r=TB))
                cj = s_ * NJ + j
                h_ = hc[s_]
                k.ts("dve", h_[:, :, :], u_[:, :, 0:128], dw[:, cj, 0:1], db[:, cj:cj + 1], op0=ALU.mult, op1=ALU.add)
                k.stt("dve", h_[:, :, :], u_[:, :, 1:129], dw[:, cj, 1:2], h_[:, :, :], ALU.mult, ALU.add)
                k.stt("dve", h_[:, :, :], u_[:, :, 2:130], dw[:, cj, 2:3], h_[:, :, :], ALU.mult, ALU.add)
            k.act(sgt[:, :, :], hc[0][:, :, :], AF.Silu)
            k.tt("pool", v3(gT, gT.t[:, j, :], r=TB), sgt[:, :, :], hc[1][:, :, :], ALU.mult)
        for u in range(TB):
            xo = xob[u % 2]
            for h in range(2):
                for j in range(NJ):
                    k.mm(po[h][:, :], gT[:, j, 128 * u:128 * u + 128], wd[:, j, 512 * h:512 * h + 512],
                         start=(j == 0), stop=(j == NJ - 1))
                k.tt("dve", xo[:, 512 * h:512 * h + 512], xts[u][:, 512 * h:512 * h + 512], po[h][:, :], ALU.add)
            if final:
                rms_stat(k, xo[:, :], D, work["junk"][:, :], st)
                k.ts("dve", xo[:, :], xo[:, :], st[:, 0:1], None, op0=ALU.mult)
                k.tt("dve", xo[:, :], xo[:, :], fg[:, :], ALU.mult)
            k.dma("pool", out_d[TB * blk + u, :, :], xo[:, :], is_output=True)
    return k.finish()


def run_F(x2_own, P, l, final):
    maps = []
    xs = {}
    for b in range(2):
        a = np.stack([x2_own[4 * b + sq] for sq in range(4)])
        xs[b] = a.transpose(1, 0, 2, 3).reshape(S, D)
    dw = np.ascontiguousarray(P["ffn_dw_w"][l].T.reshape(2 * NJ, 128, 3).transpose(1, 0, 2))
    db = np.ascontiguousarray(P["ffn_dw_b"][l].reshape(2 * NJ, 128).T)
    for c in range(NCORES):
        b, sq = c // 4, c % 4
        xh = np.zeros((NT // 4, 8, D), np.float32)
        for i in range(NT):
            t0 = 128 * (4 * i + sq)
            if t0 >= 2:
                xh[i // 4, 2 * (i % 4):2 * (i % 4) + 2] = xs[b][t0 - 2:t0]
        maps.append({"x": x2_own[c], "xh": xh, "gf": gcol_of(P["ffn_norm_g"][l]),
                     "w_up": np.ascontiguousarray(P["ffn_w_up"][l]), "w_down": np.ascontiguousarray(P["ffn_w_down"][l]),
                     "dw": dw, "db": db, "ident": IDENT, "fg": bcast128(P["final_norm_g"])})
    name = "F1" if final else "F0"
    return _run(name, lambda: build_F(final), maps)


def kernel(**inputs):
    P = {k_: np.asarray(v, dtype=np.float32) for k_, v in inputs.items()}
    x_own = [own_tiles(P["x"], c) for c in range(NCORES)]
    for l in range(2):
        resA = run_A_own(x_own, P["w_in"][l], P["mix_norm_g"][l])
        resN = run_N(resA, P["cmp_pe"][l], P["cmp_w1"][l], P["cmp_w2"][l])
        resM = run_M(np.stack(x_own), resA, resN, P, l)
        resX = run_X([r["x1"] for r in resM], P, l)
        resF = run_F([r["x2"] for r in resX], P, l, final=(l == 1))
        x_own = [np.asarray(r["xo"]) for r in resF]
    out = np.zeros((2, S, D), np.float32)
    for c in range(NCORES):
        b, sq = c // 4, c % 4
        out[b].reshape(64, 128, D)[sq::4] = x_own[c]
    return out


def run_A_own(x_own, w_in_l, g_l):
    maps = []
    for c in range(NCORES):
        maps.append({"x": np.ascontiguousarray(x_own[c]), "w_in": np.ascontiguousarray(w_in_l), "gcol": gcol_of(g_l),
                     "ident": IDENT})
    return _run("A", build_A, maps)
```
